# Optimizing a Trainium2 kernel written in Bass

```python
import functools
import jax, jax.numpy as jnp
from jax import lax
import numpy as np

D_MODEL = 2048
BATCH = 4
SEQ = 8192
DEPTH = 1
DEC_BATCH = 16
DEC_SEQ = 64
PAST_LEN = 4096

CHUNK = 64
N_META = 16
MIX_WIDTH = D_MODEL
HEAD_DIM = 64
W_A = MIX_WIDTH // 2
W_B = MIX_WIDTH - W_A
H_A = W_A // HEAD_DIM
H_B = W_B // HEAD_DIM
R_DECAY = 64
R_A = 64
R_GATE = 160
W_SHIFT = 3 * W_A + R_DECAY + R_A + R_GATE
P_COLS = W_SHIFT + 3 * W_B + 2 * D_MODEL
SHIFT_SPLITS = (W_A, 2 * W_A, 3 * W_A, 3 * W_A + R_DECAY, 3 * W_A + R_DECAY + R_A)
PROJ_SPLITS = (W_SHIFT, W_SHIFT + W_B, W_SHIFT + 2 * W_B, W_SHIFT + 3 * W_B)
SB_BLOCK = 128
SB_SCALE = HEAD_DIM ** -0.5
N_GROUPS = 4
EXPERTS_PER_GROUP = 8
N_EXPERTS = N_GROUPS * EXPERTS_PER_GROUP
TOP_K = 2
D_EXPERT = D_MODEL // 4
ALPHA = (2 * DEPTH) ** 0.25
BETA = (8 * DEPTH) ** -0.25
LN_EPS = 1e-5
GN_EPS = 64e-5

kernel_name = 'rwkv7_stickbreak_hmoe_stream_step'


def layer_norm(x, g, b):
    xf = x.astype(jnp.float32)
    mu = jnp.mean(xf, axis=-1, keepdims=True)
    var = jnp.mean(jnp.square(xf - mu), axis=-1, keepdims=True)
    return (xf - mu) * lax.rsqrt(var + LN_EPS) * g + b


def rwkv_inputs(p, prev, mu, w0, w_up, a0, a_up, g_up, k_k, k_a):
    bsz, t = p.shape[:2]
    p = p.astype(jnp.float32)
    shifted = jnp.concatenate([prev.astype(jnp.float32), p[:, :-1]], axis=1)
    xm = p + (shifted - p) * mu
    r, k, v, xw, xa, xg = jnp.split(xm, SHIFT_SPLITS, axis=-1)
    w = -jax.nn.softplus(-(w0 + jnp.tanh(xw) @ w_up)) - 0.5
    decay = jnp.exp(-jnp.exp(w))
    a = jax.nn.sigmoid(a0 + xa @ a_up)
    g = jax.nn.sigmoid(xg) @ g_up
    heads = lambda z: z.reshape(bsz, t, H_A, HEAD_DIM).astype(jnp.float32)
    kk = heads(k * k_k)
    kk = kk / jnp.maximum(jnp.linalg.norm(kk, axis=-1, keepdims=True), 1e-12)
    k = k * (1.0 + (a - 1.0) * k_a)
    return heads(r), heads(decay), heads(k), heads(v), kk, heads(a), g


def rwkv_step(state, inp):
    r, decay, k, v, kk, a = inp
    sa = jnp.einsum('bhvk,bhk->bhv', state, -kk)
    state = (state * decay[:, :, None, :] + sa[..., None] * (kk * a)[:, :, None, :]
             + v[..., None] * k[:, :, None, :])
    return state, jnp.einsum('bhvk,bhk->bhv', state, r)


def rwkv_scan(state0, r, decay, k, v, kk, a):
    xs = tuple(jnp.moveaxis(z, 1, 0) for z in (r, decay, k, v, kk, a))
    state, y = lax.scan(rwkv_step, state0.astype(jnp.float32), xs)
    return state, jnp.moveaxis(y, 0, 1)


def rwkv_output(y, r, k, v, g, r_k, lnx_g, lnx_b):
    bsz, t = y.shape[:2]
    mu = jnp.mean(y, axis=-1, keepdims=True)
    var = jnp.mean(jnp.square(y - mu), axis=-1, keepdims=True)
    yn = ((y - mu) * lax.rsqrt(var + GN_EPS)).reshape(bsz, t, W_A) * lnx_g + lnx_b
    bonus = (jnp.sum(r * k * r_k, axis=-1, keepdims=True) * v).reshape(bsz, t, W_A)
    return (yn + bonus) * g


def stick_breaking(q, k, v, q_pos, k_pos):
    z = jnp.einsum('bqhe,bkhe->bhqk', q, k).astype(jnp.float32) * SB_SCALE
    visible = k_pos[None, :] < q_pos[:, None]
    log_beta = jax.nn.log_sigmoid(z)
    log_rest = jnp.where(visible, log_beta - z, 0.0)
    log_between = lax.cumsum(log_rest, axis=3, reverse=True) - log_rest
    weight = jnp.where(visible, jnp.exp(log_beta + log_between), 0.0)
    return jnp.einsum('bhqk,bkhe->bqhe', weight, v.astype(jnp.float32))


def sb_prompt(q, k, v):
    bsz, t = q.shape[:2]
    n_blk = -(-t // SB_BLOCK)
    padw = ((0, 0), (0, n_blk * SB_BLOCK - t), (0, 0), (0, 0))
    qp, kp, vp = jnp.pad(q, padw), jnp.pad(k, padw), jnp.pad(v, padw)
    k_pos = jnp.arange(n_blk * SB_BLOCK)

    def block(i):
        start = i * SB_BLOCK
        qb = lax.dynamic_slice_in_dim(qp, start, SB_BLOCK, axis=1)
        return stick_breaking(qb, kp, vp, start + jnp.arange(SB_BLOCK), k_pos)

    o = lax.map(block, jnp.arange(n_blk))
    return jnp.moveaxis(o, 0, 1).reshape(bsz, n_blk * SB_BLOCK, H_B, HEAD_DIM)[:, :t]


def sb_with_cache(q, k, v, cache_k, cache_v):
    past, t = cache_k.shape[1], q.shape[1]
    k_all = jnp.concatenate([cache_k.astype(k.dtype), k], axis=1)
    v_all = jnp.concatenate([cache_v.astype(v.dtype), v], axis=1)
    return stick_breaking(q, k_all, v_all, past + jnp.arange(t), jnp.arange(past + t))


def token_mixer(u, prev_row, state0, sb_fn, w_in, mu, w0, w_up, a0, a_up, g_up, k_k, k_a,
                r_k, lnx_g, lnx_b, w_branch, w_out):
    bsz, t = u.shape[:2]
    proj = u @ w_in
    p_rwkv, q, k, v, gates = jnp.split(proj, PROJ_SPLITS, axis=-1)
    r, decay, kr, vr, kk, a, g = rwkv_inputs(p_rwkv, prev_row, mu, w0, w_up, a0, a_up, g_up, k_k, k_a)
    state, y = rwkv_scan(state0, r, decay, kr, vr, kk, a)
    o_a = rwkv_output(y, r, kr, vr, g, r_k, lnx_g, lnx_b)
    qh, kh, vh = (z.reshape(bsz, t, H_B, HEAD_DIM) for z in (q, k, v))
    o_b = sb_fn(qh, kh, vh).reshape(bsz, t, W_B)
    gate_a, gate_b = jnp.split(jax.nn.sigmoid(gates.astype(jnp.float32)), 2, axis=-1)
    merged = gate_a * (o_a @ w_branch[:W_A]) + gate_b * (o_b @ w_branch[W_A:])
    return merged @ w_out, kh, vh, state, p_rwkv[:, -1:]


def hier_moe(x, wg, bg, we, be, w_gate, w_up, w_down):
    n = x.shape[0]
    p_group = jax.nn.softmax((x @ wg + bg).astype(jnp.float32), axis=-1)
    g_idx = jnp.argmax(p_group, axis=-1)
    p_g = jnp.take_along_axis(p_group, g_idx[:, None], axis=-1)
    e_logits = (x @ we + be).reshape(n, N_GROUPS, EXPERTS_PER_GROUP)
    e_logits = jnp.take_along_axis(e_logits, g_idx[:, None, None], axis=1)[:, 0]
    top_p, top_i = lax.top_k(jax.nn.softmax(e_logits.astype(jnp.float32), axis=-1), TOP_K)
    weights = p_g * top_p / jnp.sum(top_p, axis=-1, keepdims=True)
    expert_id = g_idx[:, None] * EXPERTS_PER_GROUP + top_i
    combine = jnp.einsum('nk,nke->ne', weights, jax.nn.one_hot(expert_id, N_EXPERTS, dtype=jnp.float32))
    out = jnp.zeros((n, x.shape[1]), jnp.float32)
    for e in range(N_EXPERTS):
        h = jax.nn.silu(x @ w_gate[e]) * (x @ w_up[e])
        out = out + combine[:, e:e + 1] * (h @ w_down[e])
    return out


def setup_inputs(seed: int = 0) -> dict:
    key = jax.random.key(seed)
    ks = jax.random.split(key, 40)
    f32 = jnp.float32

    def nrm(i, shape, scale):
        return jax.random.normal(ks[i], shape, f32) * scale

    def unif(i, shape, lo, hi):
        return jax.random.uniform(ks[i], shape, f32, lo, hi)

    L, D = DEPTH, D_MODEL
    col_scale = (jnp.ones((P_COLS,), f32).at[2 * W_A:3 * W_A].multiply(BETA)
                 .at[PROJ_SPLITS[2]:PROJ_SPLITS[3]].multiply(BETA))
    return {
        'x_prompt': nrm(0, (BATCH, SEQ, D), 1.0),
        'x_sample': nrm(1, (DEC_BATCH, DEC_SEQ, D), 1.0),
        'cache_sb_k': nrm(2, (L, DEC_BATCH, PAST_LEN, H_B, HEAD_DIM), 1.0),
        'cache_sb_v': nrm(3, (L, DEC_BATCH, PAST_LEN, H_B, HEAD_DIM), BETA),
        'state_rwkv': nrm(4, (L, DEC_BATCH, H_A, HEAD_DIM, HEAD_DIM), 1.0),
        'state_rwkv_shift': nrm(5, (L, DEC_BATCH, 1, W_SHIFT), 1.0),
        'meta_tokens': nrm(6, (N_META, D), 1.0),
        'ln_in_g': 1.0 + nrm(7, (D,), 0.02),
        'ln_in_b': nrm(8, (D,), 0.02),
        'w_in': nrm(9, (L, D, P_COLS), D ** -0.5) * col_scale,
        'rwkv_mu': unif(10, (L, W_SHIFT), 0.0, 1.0),
        'rwkv_w0': unif(11, (L, W_A), -6.0, 1.0),
        'rwkv_w_up': nrm(12, (L, R_DECAY, W_A), 0.1),
        'rwkv_a0': nrm(13, (L, W_A), 0.1),
        'rwkv_a_up': nrm(14, (L, R_A, W_A), R_A ** -0.5),
        'rwkv_g_up': nrm(15, (L, R_GATE, W_A), R_GATE ** -0.5),
        'rwkv_k_k': 0.85 + nrm(16, (L, W_A), 0.02),
        'rwkv_k_a': 1.0 + nrm(17, (L, W_A), 0.02),
        'rwkv_r_k': nrm(18, (L, H_A, HEAD_DIM), 0.1),
        'rwkv_lnx_g': 1.0 + nrm(19, (L, W_A), 0.02),
        'rwkv_lnx_b': nrm(20, (L, W_A), 0.02),
        'w_branch': nrm(21, (L, MIX_WIDTH, D), MIX_WIDTH ** -0.5 * BETA),
        'w_out': nrm(22, (L, D, D), D ** -0.5 * BETA),
        'ln_mix_g': 1.0 + nrm(23, (L, D), 0.02),
        'ln_mix_b': nrm(24, (L, D), 0.02),
        'router_group_w': nrm(25, (L, D, N_GROUPS), D ** -0.5),
        'router_group_b': nrm(26, (L, N_GROUPS), 0.01),
        'router_expert_w': nrm(27, (L, D, N_EXPERTS), D ** -0.5),
        'router_expert_b': nrm(28, (L, N_EXPERTS), 0.01),
        'moe_w_gate': nrm(29, (L, N_EXPERTS, D, D_EXPERT), D ** -0.5),
        'moe_w_up': nrm(30, (L, N_EXPERTS, D, D_EXPERT), D ** -0.5),
        'moe_w_down': nrm(31, (L, N_EXPERTS, D_EXPERT, D), D_EXPERT ** -0.5 * BETA),
        'ln_ffn_g': 1.0 + nrm(32, (L, D), 0.02),
        'ln_ffn_b': nrm(33, (L, D), 0.02),
    }


def reference(x_prompt, x_sample, cache_sb_k, cache_sb_v, state_rwkv, state_rwkv_shift,
              meta_tokens, ln_in_g, ln_in_b, w_in, rwkv_mu, rwkv_w0, rwkv_w_up, rwkv_a0,
              rwkv_a_up, rwkv_g_up, rwkv_k_k, rwkv_k_a, rwkv_r_k, rwkv_lnx_g, rwkv_lnx_b,
              w_branch, w_out, ln_mix_g, ln_mix_b, router_group_w, router_group_b,
              router_expert_w, router_expert_b, moe_w_gate, moe_w_up, moe_w_down,
              ln_ffn_g, ln_ffn_b):
    bp = x_prompt.shape[0]
    bs, ts = x_sample.shape[:2]
    meta = jnp.broadcast_to(meta_tokens.astype(x_prompt.dtype)[None], (bp, N_META, D_MODEL))
    h_p = layer_norm(jnp.concatenate([meta, x_prompt], axis=1), ln_in_g, ln_in_b)
    h_s = layer_norm(x_sample, ln_in_g, ln_in_b)
    kp_l, vp_l, sp_l, shp_l, ks_l, vs_l, ss_l, shs_l = [], [], [], [], [], [], [], []
    for l in range(DEPTH):
        rw = (rwkv_mu[l], rwkv_w0[l], rwkv_w_up[l], rwkv_a0[l], rwkv_a_up[l], rwkv_g_up[l],
              rwkv_k_k[l], rwkv_k_a[l], rwkv_r_k[l], rwkv_lnx_g[l], rwkv_lnx_b[l])
        zero_prev = jnp.zeros((bp, 1, W_SHIFT), jnp.float32)
        zero_state = jnp.zeros((bp, H_A, HEAD_DIM, HEAD_DIM), jnp.float32)
        mix_p, k_p, v_p, st_p, sh_p = token_mixer(h_p, zero_prev, zero_state, sb_prompt,
                                                  w_in[l], *rw, w_branch[l], w_out[l])
        sb_cached = functools.partial(sb_with_cache, cache_k=cache_sb_k[l], cache_v=cache_sb_v[l])
        mix_s, k_s, v_s, st_s, sh_s = token_mixer(h_s, state_rwkv_shift[l], state_rwkv[l], sb_cached,
                                                  w_in[l], *rw, w_branch[l], w_out[l])
        h_p = layer_norm(ALPHA * h_p + mix_p, ln_mix_g[l], ln_mix_b[l])
        h_s = layer_norm(ALPHA * h_s + mix_s, ln_mix_g[l], ln_mix_b[l])
        if l == DEPTH - 1:
            h_p = h_p[:, N_META:]
        tp = h_p.shape[1]
        flat = jnp.concatenate([h_p.reshape(-1, D_MODEL), h_s.reshape(-1, D_MODEL)], axis=0)
        ffn = hier_moe(flat, router_group_w[l], router_group_b[l], router_expert_w[l],
                       router_expert_b[l], moe_w_gate[l], moe_w_up[l], moe_w_down[l])
        flat = layer_norm(ALPHA * flat + ffn, ln_ffn_g[l], ln_ffn_b[l])
        h_p = flat[:bp * tp].reshape(bp, tp, D_MODEL)
        h_s = flat[bp * tp:].reshape(bs, ts, D_MODEL)
        kp_l.append(k_p); vp_l.append(v_p); sp_l.append(st_p); shp_l.append(sh_p)
        ks_l.append(k_s); vs_l.append(v_s); ss_l.append(st_s); shs_l.append(sh_s)
    return (h_p, h_s, jnp.stack(kp_l), jnp.stack(vp_l), jnp.stack(sp_l), jnp.stack(shp_l),
            jnp.stack(ks_l), jnp.stack(vs_l), jnp.stack(ss_l), jnp.stack(shs_l))
```

```python
import contextlib
import numpy as np
import concourse.bass as bass
import concourse.mybir as mybir
from concourse.bass_utils import run_bass_kernel_spmd

F32 = mybir.dt.float32
BF16 = mybir.dt.bfloat16
AF = mybir.ActivationFunctionType
ALU = mybir.AluOpType
AX = mybir.AxisListType

FULL = dict(D=2048, SEQ=8192, PAST=4096)
NMETA, DS, HD, RD, RA, RG = 16, 64, 64, 64, 64, 160
NG, EPG, NE = 4, 8, 32
FP = 112
LN_EPS, GN_EPS = 1e-5, 64e-5
ALPHA = 2 ** 0.25
SB_SCALE = HD ** -0.5


class Buf:
    __slots__ = ("name", "w", "r", "dsem")

    def __init__(self, name):
        self.name = name
        self.w = {}
        self.r = {}
        self.dsem = None


class _Rec:
    def __getattr__(self, name):
        def f(*a, **k):
            self.call = (name, a, k)
            return self
        return f


def _record(fn):
    r = _Rec()
    fn(r)
    return r.call


class Plan:
    ENGS = ("pe", "act", "dve", "pool", "sp")

    def __init__(self, nc):
        self.nc = nc
        self.ops = {e: [] for e in self.ENGS}
        self.tick = {e: 0 for e in self.ENGS}
        self.seen = {e: {} for e in self.ENGS}
        self.dma_cnt = {}
        self.n_dsem = 0
        self.phase_dsem = 0
        self.recycle = True
        self.sems = {}
        self.free_dsems = []

    def buf(self, name="b"):
        return Buf(name)

    def bufs(self, n, name="b"):
        return [Buf(name) for _ in range(n)]

    def _deps(self, eng, reads, writes):
        need = {}
        for b in reads:
            for k, v in b.w.items():
                if need.get(k, 0) < v:
                    need[k] = v
        for b in writes:
            for k, v in b.w.items():
                if need.get(k, 0) < v:
                    need[k] = v
            for k, v in b.r.items():
                if need.get(k, 0) < v:
                    need[k] = v
        seen = self.seen[eng]
        waits = []
        for k, v in need.items():
            if seen.get(k, 0) < v:
                seen[k] = v
                waits.append((k, v))
        return waits

    def _commit(self, key, val, reads, writes):
        for b in reads:
            if b.r.get(key, 0) < val:
                b.r[key] = val
        for b in writes:
            b.w = {key: val}
            b.r = {}

    def op(self, eng, fn, reads=(), writes=()):
        waits = self._deps(eng, reads, writes)
        call = _record(fn)
        self.tick[eng] += 1
        key = "T" + eng
        self.ops[eng].append((waits, call, key, 1))
        self._commit(key, self.tick[eng], reads, writes)

    def dma(self, fn, reads=(), writes=(), sem=None, eng="sp"):
        if sem.dsem is None:
            sem.dsem = "D%d" % self.phase_dsem
            self.phase_dsem += 1
            self.n_dsem = max(self.n_dsem, self.phase_dsem)
        key = sem.dsem
        waits = self._deps(eng, reads, writes)
        val = self.dma_cnt.get(key, 0) + 16
        self.dma_cnt[key] = val
        self.ops[eng].append((waits, _record(fn), key, 16))
        self._commit(key, val, reads, writes)

    def barrier(self):
        if self.recycle:
            self.phase_dsem = 0
        need = {"T" + e: self.tick[e] for e in self.ENGS if self.tick[e] > 0}
        need.update(self.dma_cnt)
        for e in self.ENGS:
            seen = self.seen[e]
            waits = []
            for k, v in need.items():
                if seen.get(k, 0) < v:
                    seen[k] = v
                    waits.append((k, v))
            if waits:
                self.ops[e].append((waits, None, None, 0))

    def emit(self):
        nc = self.nc
        keys = ["T" + e for e in self.ENGS] + ["D%d" % i for i in range(self.n_dsem)]
        with contextlib.ExitStack() as st:
            for k in keys:
                self.sems[k] = st.enter_context(nc.semaphore(k))
            block = st.enter_context(nc.Block())

            import bisect
            targets = {"T" + e: set() for e in self.ENGS}
            for e in self.ENGS:
                for waits, fn, key, inc in self.ops[e]:
                    for (k, v) in waits:
                        if k in targets:
                            targets[k].add(v)
            ranks = {k: sorted(v) for k, v in targets.items()}

            def cval(k, v):
                if False and k in ranks:
                    return bisect.bisect_right(ranks[k], v)
                return v

            def run(eng_name):
                def body(eng):
                    t = 0
                    mykey = "T" + eng_name
                    for waits, fn, key, inc in self.ops[eng_name]:
                        for (k, v) in waits:
                            eng.wait_ge(self.sems[k], cval(k, v))
                        if fn is not None:
                            nm, a, k = fn
                            ins = getattr(eng, nm)(*a, **k)
                            if key == mykey:
                                t += 1
                                if True or t in targets[mykey]:
                                    ins.then_inc(self.sems[key], 1)
                            else:
                                ins.then_inc(self.sems[key], inc)
                return body

            block.tensor(run("pe"))
            block.scalar(run("act"))
            block.vector(run("dve"))
            block.gpsimd(run("pool"))
            block.sync(run("sp"))


class Arena:
    def __init__(self, t, n):
        self.t, self.n, self.off = t, n, 0

    def reset(self):
        self.off = 0

    def get(self, n):
        a = self.t[:, self.off:self.off + n]
        self.off += n
        assert self.off <= self.n, (self.off, self.n)
        return a


def make_consts():
    c = {}
    c["ident"] = np.eye(128, dtype=np.float32)
    j = np.arange(128)[:, None]
    k = np.arange(128)[None, :]
    c["tri"] = (j > k).astype(np.float32)
    q = np.arange(512)[None, :]
    for d in range(4):
        c["dm%d" % d] = ((128 * d + j) < q).astype(np.float32)
    qs = (np.arange(512) % 64)[None, :]
    c["dms"] = ((j < qs) & (j < 64)).astype(np.float32)
    c["pm"] = (np.arange(128) >= FP).astype(np.float32)[:, None] * np.ones((1, 2), np.float32)
    c["pmf"] = (j >= FP).astype(np.float32) * np.ones((1, 512), np.float32)
    c["dm0p"] = c["dm0"] * c["pmf"]
    c["ones"] = np.ones((128, 128), np.float32)
    c["bones"] = ((j // 64) == (k // 64)).astype(np.float32)
    s = (np.arange(128) % 64)[:, None]
    t = (np.arange(128) % 64)[None, :]
    isr = (np.arange(128) >= 64)[None, :]
    c["ma"] = np.where(isr, s <= t, s < t).astype(np.float32)
    c["mab"] = (t < s).astype(np.float32)
    c["rst"] = ((np.arange(512) % 64) != 0).astype(np.float32)[None, :] * np.ones((128, 1), np.float32)
    names = list(c)
    offs, o = {}, 0
    for n in names:
        offs[n] = (o, c[n].shape[1])
        o += c[n].shape[1]
    arr = np.concatenate([c[n] for n in names], axis=1).astype(np.float32)
    return arr, offs


CONST_ARR, CONST_OFF = make_consts()


def build(cfg, upto="D"):
    D, SEQ, PAST = cfg["D"], cfg["SEQ"], cfg["PAST"]
    WA = D // 2; WB = D - WA; HA = WA // HD; HB = WB // HD
    WS = 3 * WA + RD + RA + RG
    PC = WS + 3 * WB + 2 * D
    DE = D // 4
    KD = D // 128
    TP = NMETA + SEQ
    RP = FP + TP
    assert RP % 128 == 0
    NR = RP + 128
    NPT = RP // 128
    groups = []
    r = 0
    while r < RP:
        n = min(512, RP - r)
        groups.append((r, n, False)); r += n
    groups.append((RP, 128, True))

    nc = bass.Bass("TRN2", target_bir_lowering=False)
    P = Plan(nc)

    def din(name, shape, dt=F32):
        return nc.dram_tensor(name, list(shape), dt, kind="ExternalInput").ap()

    def dout(name, shape):
        return nc.dram_tensor(name, list(shape), F32, kind="ExternalOutput").ap()

    def dscr(name, shape, dt):
        return nc.dram_tensor(name, list(shape), dt, kind="Internal").ap()

    xp = din("xp", [SEQ, D]); meta = din("meta", [NMETA, D]); xs = din("xs", [128, D])
    ck = din("ck", [2, PAST, WB]); cv = din("cv", [2, PAST, WB])
    st_in = din("st", [2, HA, HD, HD]); shf = din("shf", [2, WS])
    w_in = din("w_in", [D, PC]); mu = din("mu", [WS]); w0 = din("w0", [WA])
    w_up = din("w_up", [RD, WA]); a0 = din("a0", [WA]); a_up = din("a_up", [RA, WA])
    g_up = din("g_up", [RG, WA]); k_k = din("k_k", [WA]); k_a = din("k_a", [WA])
    r_k = din("r_k", [WA]); lnx_g = din("lnx_g", [WA]); lnx_b = din("lnx_b", [WA])
    w_branch = din("w_branch", [D, D]); w_out = din("w_out", [D, D])
    lnv = {n: din(n, [128, D]) for n in ("ln_in_g", "ln_in_b", "ln_mix_g", "ln_mix_b", "ln_ffn_g", "ln_ffn_b")}
    wr = din("wr", [D, NG + NE]); br = din("br", [128, NG + NE])
    mwg = din("mwg", [NE, D, DE]); mwu = din("mwu", [NE, D, DE]); mwd = din("mwd", [NE, DE, D])
    cst = din("cst", list(CONST_ARR.shape))

    y_p = dout("y_p", [SEQ, D]); y_s = dout("y_s", [128, D])
    kp = dout("kp", [TP, WB]); vp = dout("vp", [TP, WB])
    stp = dout("stp", [HA, HD, HD]); shp = dout("shp", [WS])
    ks = dout("ks", [128, WB]); vs = dout("vs", [128, WB])
    sts = dout("sts", [2, HA, HD, HD]); shs = dout("shs", [2, WS])

    H = dscr("H", [NR, D], F32)
    XM = dscr("XM", [3 * WA, NR], F32)
    LW = dscr("LW", [128, NR], BF16)
    SG = dscr("SG", [RG, NR], BF16)
    QT = dscr("QT", [WB, NR], BF16); KT = dscr("KT", [WB, NR], BF16)
    VT = dscr("VT", [NR, WB], BF16)
    GT = dscr("GT", [2 * D, NR], BF16)
    OA = dscr("OA", [WA, NR], BF16); OB = dscr("OB", [WB, NR], BF16)

    es = contextlib.ExitStack()
    A32 = Arena(es.enter_context(nc.sbuf_tensor("a32", [128, 24 * 1024], F32)), 24 * 1024)
    A16 = Arena(es.enter_context(nc.sbuf_tensor("a16", [128, 40 * 1024], BF16)), 40 * 1024)
    CS = es.enter_context(nc.sbuf_tensor("cs", [128, CONST_ARR.shape[1]], F32))
    CSB = es.enter_context(nc.sbuf_tensor("csb", [128, CONST_ARR.shape[1]], BF16))
    PS = [es.enter_context(nc.psum_tensor("ps%d" % i, [128, 512], F32)) for i in range(8)]
    bPS = P.bufs(8, "ps")
    bcs = P.buf("cs")
    P.dma(lambda e: e.dma_start(out=CS[:], in_=cst), writes=[bcs], sem=bcs)
    P.op("dve", lambda e: e.tensor_copy(out=CSB[:], in_=CS[:]), reads=[bcs], writes=[bcs])

    def C(name, bf=False, rows=128):
        o, n = CONST_OFF[name]
        return (CSB if bf else CS)[0:rows, o:o + n]

    def bvec(v, n):
        return v

    def colvec(v, lo, n):
        return v[lo:lo + n].rearrange("(p o) -> p o", o=1)

    psrot = [0]

    def nextps():
        i = psrot[0] % 8
        psrot[0] += 1
        return PS[i], bPS[i]

    def layer_norm(xt, bx, gt, bt_, bgb, out, bout, stat, bstat, tmp, btmp, eng2="pool"):
        nchunk = max(1, D // 512)
        for c in range(nchunk):
            P.op("dve", lambda e, c=c: e.bn_stats(out=stat[:, 6 * c:6 * c + 6], in_=xt[:, c * (D // nchunk):(c + 1) * (D // nchunk)]),
                 reads=[bx], writes=[bstat])
        P.op("dve", lambda e: e.bn_aggr(out=stat[:, 32:34], in_=stat[:, 0:6 * nchunk]), reads=[bstat], writes=[bstat])
        P.op("dve", lambda e: e.tensor_scalar(out=stat[:, 34:35], in0=stat[:, 33:34], scalar1=LN_EPS, scalar2=None, op0=ALU.add), reads=[bstat], writes=[bstat])
        P.op("act", lambda e: e.activation(out=stat[:, 34:35], in_=stat[:, 34:35], func=AF.Ln), reads=[bstat], writes=[bstat])
        P.op("act", lambda e: e.activation(out=stat[:, 34:35], in_=stat[:, 34:35], func=AF.Exp, scale=-0.5), reads=[bstat], writes=[bstat])
        P.op("dve", lambda e: e.tensor_tensor(out=stat[:, 35:36], in0=stat[:, 32:33], in1=stat[:, 34:35], op=ALU.mult), reads=[bstat], writes=[bstat])
        P.op("dve", lambda e: e.tensor_scalar(out=stat[:, 35:36], in0=stat[:, 35:36], scalar1=-1.0, scalar2=None, op0=ALU.mult), reads=[bstat], writes=[bstat])
        P.op("act", lambda e: e.activation(out=tmp, in_=xt, func=AF.Identity, scale=stat[:, 34:35], bias=stat[:, 35:36]),
             reads=[bx, bstat], writes=[btmp])
        P.op(eng2, lambda e: e.tensor_tensor(out=tmp, in0=tmp, in1=gt, op=ALU.mult), reads=[btmp, bgb], writes=[btmp])
        P.op(eng2, lambda e: e.tensor_tensor(out=out, in0=tmp, in1=bt_, op=ALU.add), reads=[btmp, bgb], writes=[bout])

    A32.reset(); A16.reset()
    g_in = A32.get(D); b_in = A32.get(D); bgb = P.buf("gb")
    P.dma(lambda e: e.dma_start(out=g_in, in_=bvec(lnv["ln_in_g"], D)), writes=[bgb], sem=bgb)
    P.dma(lambda e: e.dma_start(out=b_in, in_=bvec(lnv["ln_in_b"], D)), writes=[bgb], sem=bgb)
    xt2 = [A32.get(D) for _ in range(2)]; bxt = P.bufs(2, "xt")
    ht2 = [A32.get(D) for _ in range(2)]; bht = P.bufs(2, "ht")
    tmpA = A32.get(D); btmpA = P.buf("tmpA")
    statA = A32.get(40); bstatA = P.buf("statA")
    hT = A16.get(KD * 512).rearrange("p (k n) -> p k n", k=KD); bhT = P.buf("hT")
    NWT = 3
    wts = [A16.get(KD * 128).rearrange("p (k m) -> p k m", k=KD) for _ in range(NWT)]; bwt = P.bufs(NWT, "wt")
    wtk = [A16.get(KD * 512).rearrange("p (k m) -> p k m", k=KD) for _ in range(2)]; bwtk = P.bufs(2, "wtk")
    psb = [A32.get(516) for _ in range(2)]; bpsb = P.bufs(2, "psb")
    xmb = [A32.get(512) for _ in range(2)]; bxmb = P.bufs(2, "xmb")
    dtmp = [A32.get(512) for _ in range(2)]; bdtmp = P.bufs(2, "dtmp")
    o16 = [A16.get(512) for _ in range(3)]; bo16 = P.bufs(3, "o16")
    o32 = [A32.get(512) for _ in range(2)]; bo32 = P.bufs(2, "o32")
    NCH_R = 3 * WA // 128
    NMU = NCH_R + 3
    mucol = A32.get(NMU); bmu = P.buf("mu")
    carry = A32.get(NMU); bcarry = P.buf("carry")
    rch = [(128 * i, 128) for i in range(NCH_R)] + [(3 * WA, 128), (3 * WA + 128, 128), (3 * WA + 256, 32)]
    for i, (c0, m) in enumerate(rch):
        P.dma(lambda e, i=i, c0=c0, m=m: e.dma_start(out=mucol[0:m, i:i + 1], in_=colvec(mu, c0, m)), writes=[bmu], sem=bmu)
    P.op("pool", lambda e: e.memset(carry, 0.0), writes=[bcarry])
    cnt = dict(t=0, w=0, wk=0, p=0, o=0, o3=0)

    for gi, (r0, n, is_s) in enumerate(groups):
        nt = n // 128
        for ti in range(nt):
            row = r0 + ti * 128
            s = cnt["t"] % 2; cnt["t"] += 1
            xt, bx, ht, bh = xt2[s], bxt[s], ht2[s], bht[s]
            if is_s:
                P.dma(lambda e, xt=xt: e.dma_start(out=xt, in_=xs), writes=[bx], sem=bx)
            elif row == 0:
                P.op("pool", lambda e, xt=xt: e.memset(xt, 0.0), writes=[bx])
                P.dma(lambda e, xt=xt: e.dma_start(out=xt[FP:128, :], in_=meta), writes=[bx], sem=bx)
            else:
                P.dma(lambda e, xt=xt, row=row: e.dma_start(out=xt, in_=xp[row - 128:row, :]), writes=[bx], sem=bx)
            layer_norm(xt, bx, g_in, b_in, bgb, ht, bh, statA, bstatA, tmpA, btmpA)
            P.dma(lambda e, ht=ht, row=row: e.dma_start(out=H[row:row + 128, :], in_=ht), reads=[bh], sem=bh)
            for kb in range(0, KD, 4):
                ps, bps = nextps()
                for k in range(kb, min(KD, kb + 4)):
                    P.op("pe", lambda e, ps=ps, k=k, kb=kb, ht=ht: e.transpose(out=ps[:, (k - kb) * 128:(k - kb + 1) * 128], in_=ht[:, k * 128:(k + 1) * 128], identity=C("ident")),
                         reads=[bh, bcs], writes=[bps])
                kk_ = min(KD, kb + 4) - kb
                P.op("act", lambda e, ps=ps, kb=kb, kk_=kk_, ti=ti: e.activation(out=hT[:, kb:kb + kk_, ti * 128:(ti + 1) * 128], in_=ps[:, 0:kk_ * 128].rearrange("p (k n) -> p k n", k=kk_), func=AF.Copy),
                     reads=[bps], writes=[bhT])
            if row == 0 and not is_s:
                P.op("pool", lambda e: e.memset(hT[:, :, 0:FP], 0.0), writes=[bhT])

        if cfg.get("cut", 9) <= 1:
            continue
        def fm_proj(c0, m, evac):
            s = cnt["w"] % NWT; cnt["w"] += 1
            wt, bw = wts[s], bwt[s]
            P.dma(lambda e: e.dma_start(out=wt[:, :, 0:m], in_=w_in[:, c0:c0 + m].rearrange("(k p) m -> p k m", p=128)),
                  writes=[bw], sem=bw, eng="pool")
            ps, bps = nextps()
            for k in range(KD):
                P.op("pe", lambda e, k=k: e.matmul(ps[0:m, 0:n], lhsT=wt[:, k, 0:m], rhs=hT[:, k, 0:n], start=(k == 0), stop=(k == KD - 1)),
                     reads=[bw, bhT], writes=[bps])
            evac(ps, bps)

        for ci, (c0, m) in enumerate(rch):
            def evac_r(ps, bps, ci=ci, c0=c0, m=m):
                s = cnt["p"] % 2; cnt["p"] += 1
                pb, bpb, xm_, bxm, dt_, bdt = psb[s], bpsb[s], xmb[s], bxmb[s], dtmp[s], bdtmp[s]
                segs = [(0, n, None)] if not is_s else [(0, 64, 0), (64, 64, 1)]
                P.op("act", lambda e: e.activation(out=pb[0:m, 1:n + 1], in_=ps[0:m, 0:n], func=AF.Copy), reads=[bps], writes=[bpb])
                if not is_s:
                    P.op("act", lambda e: e.activation(out=pb[0:m, 0:1], in_=carry[0:m, ci:ci + 1], func=AF.Copy), reads=[bcarry], writes=[bpb])
                    P.op("act", lambda e: e.activation(out=carry[0:m, ci:ci + 1], in_=pb[0:m, n:n + 1], func=AF.Copy), reads=[bpb], writes=[bcarry])
                    P.op("dve", lambda e: e.tensor_tensor(out=dt_[0:m, 0:n], in0=pb[0:m, 0:n], in1=pb[0:m, 1:n + 1], op=ALU.subtract), reads=[bpb], writes=[bdt])
                    if r0 + n == RP:
                        P.dma(lambda e: e.dma_start(out=shp[c0:c0 + m].rearrange("(p o) -> p o", o=1), in_=pb[0:m, n:n + 1]), reads=[bpb], sem=bpb)
                else:
                    P.dma(lambda e: e.dma_start(out=dt_[0:m, 0:1], in_=shf[0, c0:c0 + m].rearrange("(p o) -> p o", o=1)), writes=[bdt], sem=bdt)
                    P.dma(lambda e: e.dma_start(out=dt_[0:m, 64:65], in_=shf[1, c0:c0 + m].rearrange("(p o) -> p o", o=1)), writes=[bdt], sem=bdt)
                    P.op("act", lambda e: e.activation(out=dt_[0:m, 1:64], in_=pb[0:m, 1:64], func=AF.Copy), reads=[bpb], writes=[bdt])
                    P.op("act", lambda e: e.activation(out=dt_[0:m, 65:128], in_=pb[0:m, 65:128], func=AF.Copy), reads=[bpb], writes=[bdt])
                    P.op("dve", lambda e: e.tensor_tensor(out=dt_[0:m, 0:n], in0=dt_[0:m, 0:n], in1=pb[0:m, 1:n + 1], op=ALU.subtract), reads=[bpb, bdt], writes=[bdt])
                    P.dma(lambda e: e.dma_start(out=shs[0, c0:c0 + m].rearrange("(p o) -> p o", o=1), in_=pb[0:m, 64:65]), reads=[bpb], sem=bpb)
                    P.dma(lambda e: e.dma_start(out=shs[1, c0:c0 + m].rearrange("(p o) -> p o", o=1), in_=pb[0:m, 128:129]), reads=[bpb], sem=bpb)
                P.op("dve", lambda e: e.scalar_tensor_tensor(out=xm_[0:m, 0:n], in0=dt_[0:m, 0:n], scalar=mucol[0:m, ci:ci + 1], in1=pb[0:m, 1:n + 1], op0=ALU.mult, op1=ALU.add),
                     reads=[bdt, bpb, bmu], writes=[bxm])
                if ci < NCH_R:
                    P.dma(lambda e: e.dma_start(out=XM[c0:c0 + m, r0:r0 + n], in_=xm_[0:m, 0:n]), reads=[bxm], sem=bxm)
                else:
                    so = cnt["o"] % 3; cnt["o"] += 1
                    ob, bob = o16[so], bo16[so]
                    if ci == NCH_R:
                        P.op("act", lambda e: e.activation(out=ob[0:64, 0:n], in_=xm_[0:64, 0:n], func=AF.Tanh), reads=[bxm], writes=[bob])
                        P.op("act", lambda e: e.activation(out=ob[64:128, 0:n], in_=xm_[64:128, 0:n], func=AF.Copy), reads=[bxm], writes=[bob])
                        P.dma(lambda e: e.dma_start(out=LW[:, r0:r0 + n], in_=ob[:, 0:n]), reads=[bob], sem=bob)
                    else:
                        g0 = 0 if ci == NCH_R + 1 else 128
                        P.op("act", lambda e: e.activation(out=ob[0:m, 0:n], in_=xm_[0:m, 0:n], func=AF.Sigmoid), reads=[bxm], writes=[bob])
                        P.dma(lambda e: e.dma_start(out=SG[g0:g0 + m, r0:r0 + n], in_=ob[0:m, 0:n]), reads=[bob], sem=bob)
            fm_proj(c0, m, evac_r)

        if cfg.get("cut", 9) <= 2:
            continue
        for (base, dst, fn) in ((WS, QT, AF.Copy), (WS + WB, KT, AF.Copy)):
            for j in range(WB // 128):
                def evac_q(ps, bps, dst=dst, j=j, fn=fn):
                    so = cnt["o"] % 3; cnt["o"] += 1
                    ob, bob = o16[so], bo16[so]
                    P.op("act", lambda e: e.activation(out=ob[:, 0:n], in_=ps[:, 0:n], func=fn), reads=[bps], writes=[bob])
                    P.dma(lambda e: e.dma_start(out=dst[j * 128:(j + 1) * 128, r0:r0 + n], in_=ob[:, 0:n]), reads=[bob], sem=bob)
                fm_proj(base + 128 * j, 128, evac_q)
        for j in range(2 * D // 128):
            def evac_g(ps, bps, j=j):
                so = cnt["o"] % 3; cnt["o"] += 1
                ob, bob = o16[so], bo16[so]
                P.op("act", lambda e: e.activation(out=ob[:, 0:n], in_=ps[:, 0:n], func=AF.Sigmoid), reads=[bps], writes=[bob])
                P.dma(lambda e: e.dma_start(out=GT[j * 128:(j + 1) * 128, r0:r0 + n], in_=ob[:, 0:n]), reads=[bob], sem=bob)
            fm_proj(WS + 3 * WB + 128 * j, 128, evac_g)

        if cfg.get("cut", 9) <= 3:
            continue
        for which, base in ((0, WS + WB), (1, WS + 2 * WB)):
            for nb in range(0, WB, 512):
                nw = min(512, WB - nb)
                s = cnt["wk"] % 2; cnt["wk"] += 1
                wk_, bwk = wtk[s], bwtk[s]
                P.dma(lambda e: e.dma_start(out=wk_[:, :, 0:nw], in_=w_in[:, base + nb:base + nb + nw].rearrange("(k p) m -> p k m", p=128)),
                      writes=[bwk], sem=bwk, eng="pool")
                for ti in range(nt):
                    row = r0 + ti * 128
                    ps, bps = nextps()
                    for k in range(KD):
                        P.op("pe", lambda e, k=k, ti=ti: e.matmul(ps[:, 0:nw], lhsT=hT[:, k, ti * 128:(ti + 1) * 128], rhs=wk_[:, k, 0:nw], start=(k == 0), stop=(k == KD - 1)),
                             reads=[bwk, bhT], writes=[bps])
                    so = cnt["o3"] % 2; cnt["o3"] += 1
                    of, bof = o32[so], bo32[so]
                    P.op("act", lambda e, ps=ps, of=of: e.activation(out=of[:, 0:nw], in_=ps[:, 0:nw], func=AF.Copy), reads=[bps], writes=[bof])
                    if cfg.get("cut", 9) <= 4:
                        continue
                    if is_s:
                        dst = (ks, vs)[which]
                        P.dma(lambda e, of=of, dst=dst: e.dma_start(out=dst[:, nb:nb + nw], in_=of[:, 0:nw]), reads=[bof], sem=bof)
                    elif row == 0:
                        dst = (kp, vp)[which]
                        P.dma(lambda e, of=of, dst=dst: e.dma_start(out=dst[0:NMETA, nb:nb + nw], in_=of[FP:128, 0:nw]), reads=[bof], sem=bof)
                    else:
                        dst = (kp, vp)[which]
                        P.dma(lambda e, of=of, dst=dst, row=row: e.dma_start(out=dst[row - FP:row - FP + 128, nb:nb + nw], in_=of[:, 0:nw]), reads=[bof], sem=bof)
                    if which == 1 and cfg.get("cut", 9) > 5:
                        so = cnt["o"] % 3; cnt["o"] += 1
                        ob, bob = o16[so], bo16[so]
                        P.op("act", lambda e, ob=ob, ps=ps: e.activation(out=ob[:, 0:nw], in_=ps[:, 0:nw], func=AF.Copy), reads=[bps], writes=[bob])
                        P.dma(lambda e, ob=ob, row=row: e.dma_start(out=VT[row:row + 128, nb:nb + nw], in_=ob[:, 0:nw]), reads=[bob], sem=bob)
    P.barrier()
    if upto == "A":
        return nc, P, dict(locals()), es
    A32.reset(); A16.reset()
    dbg = dout("dbg", [WB, NR]) if cfg.get("dbg") else None
    NB = RP // 128
    kTs = A16.get(RP); qTs = A16.get(RP); bkq = P.buf("kq")
    Vs = A16.get(NB * 128).rearrange("p (b c) -> p b c", b=NB); bV = P.buf("V")
    NSB = 5
    nlrb = [A16.get(512) for _ in range(NSB)]; bnlr = P.bufs(NSB, "nlr")
    Wb = [A16.get(512) for _ in range(NSB)]; bW = P.bufs(NSB, "W")
    ob16 = [A16.get(512) for _ in range(2)]; bob16 = P.bufs(2, "ob")
    tb = [A32.get(512) for _ in range(NSB)]; btb = P.bufs(NSB, "t")
    spb = [A32.get(512) for _ in range(NSB)]; bspb = P.bufs(NSB, "sp")
    cspb = [A32.get(512) for _ in range(NSB)]; bcsp = P.bufs(NSB, "csp")
    totb = [A32.get(512) for _ in range(NSB)]; btot = P.bufs(NSB, "tot")
    Cb = [A32.get(512) for _ in range(4)]; bC = P.bufs(4, "C")
    ob32 = [A32.get(512) for _ in range(2)]; bob32 = P.bufs(2, "ob32")
    bc = dict(i=0, sweep=0)

    pipe = []

    def stZ(d):
        n, mask = d["n"], d["mask"]
        i, j = d["i2"], d["i3"]
        zps, bz = PS[i], bPS[i]
        t_, bt_, sp_, bsp_ = tb[j], btb[j], spb[j], bspb[j]
        nl, bnl = nlrb[j], bnlr[j]
        d["zmm"](zps, bz)
        P.op("act", lambda e: e.activation(out=t_[:, 0:n], in_=zps[:, 0:n], func=AF.Exp, scale=-SB_SCALE), reads=[bz], writes=[bt_])
        P.op("act", lambda e: e.activation(out=sp_[:, 0:n], in_=t_[:, 0:n], func=AF.Ln, bias=1.0), reads=[bt_], writes=[bsp_])
        P.op("dve", lambda e: e.scalar_tensor_tensor(out=nl[:, 0:n], in0=zps[:, 0:n], scalar=SB_SCALE, in1=sp_[:, 0:n], op0=ALU.mult, op1=ALU.add),
             reads=[bz, bsp_], writes=[bnl])
        if mask is not None:
            P.op("pool", lambda e: e.tensor_tensor(out=nl[:, 0:n], in0=nl[:, 0:n], in1=mask, op=ALU.mult), reads=[bnl, bcs], writes=[bnl])

    def stT(d):
        n = d["n"]
        i, j = d["i2"], d["i3"]
        cps, bcp = PS[2 + i], bPS[2 + i]
        sps, bsp = PS[4 + i], bPS[4 + i]
        sp_, bsp_ = spb[j], bspb[j]
        nl, bnl = nlrb[j], bnlr[j]
        csp, bcs_ = cspb[j], bcsp[j]
        P.op("pe", lambda e: e.matmul(cps[:, 0:n], lhsT=C("tri", True), rhs=nl[:, 0:n], start=True, stop=True), reads=[bnl, bcs], writes=[bcp])
        if not d["last"]:
            P.op("pe", lambda e: e.matmul(sps[:, 0:n], lhsT=C("ones", True), rhs=nl[:, 0:n], start=True, stop=True), reads=[bnl, bcs], writes=[bsp])
        P.op("dve", lambda e: e.tensor_tensor(out=csp[:, 0:n], in0=cps[:, 0:n], in1=sp_[:, 0:n], op=ALU.add), reads=[bcp, bsp_], writes=[bcs_])

    def stB(d):
        n, mask = d["n"], d["mask"]
        i, j = d["i2"], d["i3"]
        first, last = d["first"], d["last"]
        Cc, bCc, Cn, bCn = d["Cc"], d["bCc"], d["Cn"], d["bCn"]
        sps, bsp = PS[4 + i], bPS[4 + i]
        csp, bcs_, tot, bto = cspb[j], bcsp[j], totb[j], btot[j]
        W, bW_ = Wb[j], bW[j]
        if not last:
            if first:
                P.op("dve", lambda e: e.tensor_copy(out=Cn[:, 0:n], in_=sps[:, 0:n]), reads=[bsp], writes=[bCn])
            else:
                P.op("dve", lambda e: e.tensor_tensor(out=Cn[:, 0:n], in0=sps[:, 0:n], in1=Cc[:, 0:n], op=ALU.add), reads=[bsp, bCc], writes=[bCn])
        if first:
            src, bsrc = csp, bcs_
        else:
            P.op("pool", lambda e: e.tensor_tensor(out=tot[:, 0:n], in0=csp[:, 0:n], in1=Cc[:, 0:n], op=ALU.add), reads=[bcs_, bCc], writes=[bto])
            src, bsrc = tot, bto
        P.op("act", lambda e: e.activation(out=W[:, 0:n], in_=src[:, 0:n], func=AF.Exp, scale=-1.0), reads=[bsrc], writes=[bW_])
        if mask is not None:
            P.op("pool", lambda e: e.tensor_tensor(out=W[:, 0:n], in0=W[:, 0:n], in1=mask, op=ALU.mult), reads=[bW_, bcs], writes=[bW_])

    def stPV(d):
        j = d["i3"]
        d["pvmm"](Wb[j], bW[j], d["ops"], d["bops"], d["first"], d["last"])
        if d["finish"] is not None:
            d["finish"]()

    def sb_sweep(n, blocks, Ct, bCt, ops, bops, finish):
        nb_ = len(blocks)
        for b, (zmm, pvmm, mask) in enumerate(blocks):
            k = bc["i"]; bc["i"] += 1
            pipe.append(dict(n=n, zmm=zmm, pvmm=pvmm, mask=mask, i2=k % 2, i3=k % NSB, Cc=Ct[b % 2], bCc=bCt[b % 2], Cn=Ct[(b + 1) % 2], bCn=bCt[(b + 1) % 2],
                             ops=ops, bops=bops, first=(b == 0), last=(b == nb_ - 1), finish=(finish if b == nb_ - 1 else None)))

    def sb_flush():
        DT, DB, DP = 3, 1, 3
        L = len(pipe)
        for s_ in range(L + DT + DB + DP):
            for off, fn in ((DT + DB + DP, stPV), (DT + DB, stB), (DT, stT), (0, stZ)):
                if 0 <= s_ - off < L:
                    fn(pipe[s_ - off])
        del pipe[:]

    def sweep_out(n, ops, bops, dst_rows, col0, i):
        o_, bo_ = ob16[i], bob16[i]
        P.op("act", lambda e: e.activation(out=o_[0:64, 0:n], in_=ops[0:64, 0:n], func=AF.Copy), reads=[bops], writes=[bo_])
        P.dma(lambda e: e.dma_start(out=OB[dst_rows:dst_rows + 64, col0:col0 + n], in_=o_[0:64, 0:n]), reads=[bo_], sem=bo_)
        if dbg is not None:
            o2, bo2 = ob32[i], bob32[i]
            P.op("act", lambda e: e.activation(out=o2[0:64, 0:n], in_=ops[0:64, 0:n], func=AF.Copy), reads=[bops], writes=[bo2])
            P.dma(lambda e: e.dma_start(out=dbg[dst_rows:dst_rows + 64, col0:col0 + n], in_=o2[0:64, 0:n]), reads=[bo2], sem=bo2)

    for hp in range(HB // 2):
        P.dma(lambda e: e.dma_start(out=kTs, in_=KT[128 * hp:128 * hp + 128, 0:RP]), writes=[bkq], sem=bkq)
        P.dma(lambda e: e.dma_start(out=qTs, in_=QT[128 * hp:128 * hp + 128, 0:RP]), writes=[bkq], sem=bkq)
        P.dma(lambda e: e.dma_start(out=Vs, in_=VT[0:RP, 128 * hp:128 * hp + 128].rearrange("(b p) c -> p b c", p=128)), writes=[bV], sem=bV)
        for par in range(2):
            pb = 64 * par
            for (r0, n, is_s) in groups:
                if is_s:
                    continue
                si = bc["sweep"] % 2
                ops, bops = PS[6 + si], bPS[6 + si]
                Ct, bCt = Cb[2 * si:2 * si + 2], bC[2 * si:2 * si + 2]
                kbs = list(range((r0 + n) // 128 - 1, -1, -1))
                blks = []
                for idx, kb in enumerate(kbs):
                    dj = kb - r0 // 128
                    if kb == 0:
                        mask = C("dm0p", True)[:, 0:n] if dj == 0 else C("pmf", True)[:, 0:n]
                    elif dj >= 0:
                        mask = C("dm%d" % dj, True)[:, 0:n]
                    else:
                        mask = None

                    def zmm(zps, bz, kb=kb, n=n, r0=r0, pb=pb):
                        P.op("pe", lambda e: e.matmul(zps[:, 0:n], lhsT=kTs[pb:pb + 64, kb * 128:(kb + 1) * 128], rhs=qTs[pb:pb + 64, r0:r0 + n], start=True, stop=True),
                             reads=[bkq], writes=[bz])

                    def pvmm(W, bW_, ops, bops, first, last, kb=kb, n=n, pb=pb):
                        P.op("pe", lambda e: e.matmul(ops[0:64, 0:n], lhsT=Vs[:, kb, pb:pb + 64], rhs=W[:, 0:n], start=first, stop=last),
                             reads=[bV, bW_], writes=[bops])
                    blks.append((zmm, pvmm, mask))
                sb_sweep(n, blks, Ct, bCt, ops, bops,
                         lambda n=n, ops=ops, bops=bops, dr=(2 * hp + par) * 64, r0=r0, si=si: sweep_out(n, ops, bops, dr, r0, si))
                bc["sweep"] += 1
        sb_flush()
    P.barrier()
    A32.reset(); A16.reset()
    GH = min(8, HB); NPAIR = GH // 2; ns = GH * 64; NBc = PAST // 128
    nlrb = [A16.get(512) for _ in range(NSB)]; Wb = [A16.get(512) for _ in range(NSB)]; ob16 = [A16.get(512) for _ in range(2)]
    tb = [A32.get(512) for _ in range(NSB)]; spb = [A32.get(512) for _ in range(NSB)]; cspb = [A32.get(512) for _ in range(NSB)]
    totb = [A32.get(512) for _ in range(NSB)]; Cb = [A32.get(512) for _ in range(4)]; ob32 = [A32.get(512) for _ in range(2)]
    kTc = A16.get(NPAIR * PAST).rearrange("p (a k) -> p a k", a=NPAIR); bkTc = P.buf("kTc")
    Vc = A16.get(NBc * ns).rearrange("p (b c) -> p b c", b=NBc); bVc = P.buf("Vc")
    kTn = A16.get(NPAIR * 128).rearrange("p (a k) -> p a k", a=NPAIR); bkTn = P.buf("kTn")
    Vn = A16.get(ns); bVn = P.buf("Vn")
    qs_ = A16.get(NPAIR * 64).rearrange("p (a k) -> p a k", a=NPAIR); bqs = P.buf("qs")
    ckb = [A32.get(512) for _ in range(2)]; bckb = P.bufs(2, "ckb")
    for j in range(2):
        for g in range(HB // GH):
            c0 = g * GH * 64
            qc = RP + 64 * j
            P.dma(lambda e: e.dma_start(out=Vc, in_=cv[j, :, c0:c0 + ns].rearrange("(b p) c -> p b c", p=128)), writes=[bVc], sem=bVc, eng="pool")
            P.op("pool", lambda e: e.memset(kTn, 0.0), writes=[bkTn])
            P.op("pool", lambda e: e.memset(Vn, 0.0), writes=[bVn])
            for pi in range(NPAIR):
                P.dma(lambda e, pi=pi: e.dma_start(out=kTn[:, pi, 0:64], in_=KT[c0 + pi * 128:c0 + pi * 128 + 128, qc:qc + 64]), writes=[bkTn], sem=bkTn)
                P.dma(lambda e, pi=pi: e.dma_start(out=qs_[:, pi, :], in_=QT[c0 + pi * 128:c0 + pi * 128 + 128, qc:qc + 64]), writes=[bqs], sem=bqs)
            P.dma(lambda e: e.dma_start(out=Vn[0:64, :], in_=VT[qc:qc + 64, c0:c0 + ns]), writes=[bVn], sem=bVn)
            for kb in range(NBc):
                s2 = kb % 2
                P.dma(lambda e, kb=kb, s2=s2: e.dma_start(out=ckb[s2][:, 0:ns], in_=ck[j, kb * 128:(kb + 1) * 128, c0:c0 + ns]), writes=[bckb[s2]], sem=bckb[s2])
                ps, bps = PS[6 + s2], bPS[6 + s2]
                for pi in range(NPAIR):
                    P.op("pe", lambda e, pi=pi, s2=s2, ps=ps: e.transpose(out=ps[:, pi * 128:(pi + 1) * 128], in_=ckb[s2][:, pi * 128:(pi + 1) * 128], identity=C("ident")),
                         reads=[bckb[s2], bcs], writes=[bps])
                P.op("act", lambda e, kb=kb, ps=ps: e.activation(out=kTc[:, :, kb * 128:(kb + 1) * 128], in_=ps[:, 0:NPAIR * 128].rearrange("p (a k) -> p a k", a=NPAIR), func=AF.Copy),
                     reads=[bps], writes=[bkTc])
            si = bc["sweep"] % 2
            ops, bops = PS[6 + si], bPS[6 + si]
            Ct, bCt = Cb[2 * si:2 * si + 2], bC[2 * si:2 * si + 2]
            blocks = [("n", 0)] + [("c", kb) for kb in range(NBc - 1, -1, -1)]
            blks = []
            for idx, (kind, kb) in enumerate(blocks):
                def zmm(zps, bz, kind=kind, kb=kb):
                    for s in range(GH):
                        pi, pb = s // 2, 64 * (s % 2)
                        lh = kTn[pb:pb + 64, pi, :] if kind == "n" else kTc[pb:pb + 64, pi, kb * 128:(kb + 1) * 128]
                        P.op("pe", lambda e, s=s, lh=lh, pi=pi, pb=pb: e.matmul(zps[:, 64 * s:64 * s + 64], lhsT=lh, rhs=qs_[pb:pb + 64, pi, :], start=True, stop=True),
                             reads=[bkTn if kind == "n" else bkTc, bqs], writes=[bz])

                def pvmm(W, bW_, ops, bops, first, last, kind=kind, kb=kb):
                    for s in range(GH):
                        lh = Vn[:, 64 * s:64 * s + 64] if kind == "n" else Vc[:, kb, 64 * s:64 * s + 64]
                        P.op("pe", lambda e, s=s, lh=lh: e.matmul(ops[0:64, 64 * s:64 * s + 64], lhsT=lh, rhs=W[:, 64 * s:64 * s + 64], start=(first and s == 0), stop=last, skip_group_check=True),
                             reads=[bVn if kind == "n" else bVc, bW_], writes=[bops])
                blks.append((zmm, pvmm, C("dms", True)[:, 0:ns] if kind == "n" else None))
            sb_sweep(ns, blks, Ct, bCt, ops, bops, None)
            sb_flush()
            i = bc["sweep"] % 2
            o_, bo_ = ob16[i], bob16[i]
            P.op("act", lambda e: e.activation(out=o_[0:64, 0:ns], in_=ops[0:64, 0:ns], func=AF.Copy), reads=[bops], writes=[bo_])
            for s in range(GH):
                P.dma(lambda e, s=s: e.dma_start(out=OB[c0 + 64 * s:c0 + 64 * s + 64, qc:qc + 64], in_=o_[0:64, 64 * s:64 * s + 64]), reads=[bo_], sem=bo_)
            if dbg is not None:
                o2, bo2 = ob32[i], bob32[i]
                P.op("act", lambda e: e.activation(out=o2[0:64, 0:ns], in_=ops[0:64, 0:ns], func=AF.Copy), reads=[bops], writes=[bo2])
                for s in range(GH):
                    P.dma(lambda e, s=s: e.dma_start(out=dbg[c0 + 64 * s:c0 + 64 * s + 64, qc:qc + 64], in_=o2[0:64, 64 * s:64 * s + 64]), reads=[bo2], sem=bo2)
            bc["sweep"] += 1
    P.barrier()
    if upto == "B":
        return nc, P, dict(locals()), es
    A32.reset(); A16.reset()
    NPR = HA // 2
    KAP = float(np.exp(-0.5))
    HG = min(4, HA)
    dbg2 = dout("dbg2", [WA, NR]) if cfg.get("dbg") else None
    prm = A32.get(NPR * 8).rearrange("p (a c) -> p a c", a=NPR); bprm = P.buf("prm")
    for a in range(NPR):
        for ci, vec in enumerate((w0, a0, k_k, k_a, r_k, lnx_g, lnx_b)):
            P.dma(lambda e, a=a, ci=ci, vec=vec: e.dma_start(out=prm[:, a, ci:ci + 1], in_=colvec(vec, 128 * a, 128)), writes=[bprm], sem=bprm)
    P.op("dve", lambda e: e.tensor_scalar(out=prm[:, :, 7:8], in0=prm[:, :, 3:4], scalar1=-1.0, scalar2=1.0, op0=ALU.mult, op1=ALU.add), reads=[bprm], writes=[bprm])
    lwt = A16.get(WA); gu1 = A16.get(WA); gu2 = A16.get(WA); blw = P.buf("lw")
    P.dma(lambda e: e.dma_start(out=lwt[0:64, :], in_=w_up), writes=[blw], sem=blw, eng="pool")
    P.dma(lambda e: e.dma_start(out=lwt[64:128, :], in_=a_up), writes=[blw], sem=blw, eng="pool")
    P.dma(lambda e: e.dma_start(out=gu1, in_=g_up[0:128, :]), writes=[blw], sem=blw, eng="pool")
    P.dma(lambda e: e.dma_start(out=gu2[0:32, :], in_=g_up[128:160, :]), writes=[blw], sem=blw, eng="pool")
    brk = A16.get(NPR * 128).rearrange("p (a c) -> p a c", a=NPR); bbrk = P.buf("brk")
    bmean = A16.get(128); bbm = P.buf("bmean")
    for a in range(NPR):
        P.op("dve", lambda e, a=a: e.tensor_scalar(out=brk[:, a, :], in0=C("bones"), scalar1=prm[:, a, 4:5], scalar2=None, op0=ALU.mult), reads=[bcs, bprm], writes=[bbrk])
    P.op("dve", lambda e: e.tensor_scalar(out=bmean, in0=C("bones"), scalar1=1.0 / 64.0, scalar2=None, op0=ALU.mult), reads=[bcs], writes=[bbm])
    identb = C("ident", True)
    ST = A32.get(NPR * 64).rearrange("p (a v) -> p a v", a=NPR); bST = P.buf("ST")
    STb = A16.get(NPR * 64).rearrange("p (a v) -> p a v", a=NPR); bSTb = P.buf("STb")
    NCM = 4
    AR = A16.get(NPR * NCM * 128).rearrange("p (a c x) -> p a c x", a=NPR, c=NCM); bAR = P.buf("AR")
    BK = A16.get(NPR * NCM * 128).rearrange("p (a c x) -> p a c x", a=NPR, c=NCM); bBK = P.buf("BK")
    BT = A16.get(NCM * WA).rearrange("p (c x) -> p c x", c=NCM); bBT = P.buf("BT")
    KK = A16.get(NCM * WA).rearrange("p (c x) -> p c x", c=NCM); bKK = P.buf("KK")
    VV = A16.get(NCM * WA).rearrange("p (c x) -> p c x", c=NCM); bVV = P.buf("VV")
    PLc = A32.get(NPR * NCM).rearrange("p (a c) -> p a c", a=NPR); bPL = P.buf("PL")
    BON = A32.get(NPR * 512).rearrange("p (a n) -> p a n", a=NPR); bBON = P.buf("BON")
    GG = A32.get(NPR * 512).rearrange("p (a n) -> p a n", a=NPR); bGG = P.buf("GG")
    YT = A32.get(NPR * 512).rearrange("p (a n) -> p a n", a=NPR); bYT = P.buf("YT")
    lwin = A16.get(512); sg1 = A16.get(512); sg2 = A16.get(512); blin = P.buf("lin")
    NF = 14
    f32t = [A32.get(512) for _ in range(NF)]; bf32 = P.bufs(NF, "f")
    b16t = [A16.get(512) for _ in range(4)]; bb16 = P.bufs(4, "h")
    Amb = A16.get(HG * 128).rearrange("p (h x) -> p h x", h=HG); bAmb = P.buf("Amb")
    Amk = A16.get(HG * 128).rearrange("p (h x) -> p h x", h=HG); bAmk = P.buf("Amk")
    Xs = [A16.get(HG * 64).rearrange("p (h x) -> p h x", h=HG) for _ in range(2)]; bXs = P.bufs(2, "X")
    XTs = [A16.get(HG * 64).rearrange("p (h x) -> p h x", h=HG) for _ in range(2)]; bXTs = P.bufs(2, "XT")
    Pm = A16.get(HA * 64).rearrange("p (h x) -> p h x", h=HA); bPm = P.buf("Pm")
    Pm32 = A32.get(HG * 64).rearrange("p (h x) -> p h x", h=HG); bPm32 = P.buf("Pm32")
    AmbA = A16.get(HA * 128).rearrange("p (h x) -> p h x", h=HA); bAmbA = P.buf("AmbA")
    AmkA = A16.get(HA * 128).rearrange("p (h x) -> p h x", h=HA); bAmkA = P.buf("AmkA")
    Wt = A16.get(HA * 64).rearrange("p (h x) -> p h x", h=HA); bWt = P.buf("Wt")
    Ut = A16.get(HA * 64).rearrange("p (h x) -> p h x", h=HA); bUt = P.buf("Ut")
    stt = A32.get(HA * 64); bstt = P.buf("stt")
    oa16 = [A16.get(512) for _ in range(2)]; boa = P.bufs(2, "oa")
    fcnt = dict(f=0, h=0, o=0)

    def F():
        i = fcnt["f"] % NF; fcnt["f"] += 1
        return f32t[i], bf32[i]

    def Hh():
        i = fcnt["h"] % 4; fcnt["h"] += 1
        return b16t[i], bb16[i]

    def load_state_zero():
        P.op("pool", lambda e: e.memset(ST, 0.0), writes=[bST])
        P.op("pool", lambda e: e.memset(STb, 0.0), writes=[bSTb])

    def load_state(j):
        P.dma(lambda e: e.dma_start(out=stt[0:64, :].rearrange("p (h k) -> p h k", h=HA), in_=st_in[j].rearrange("h v k -> v h k")), writes=[bstt], sem=bstt)
        for a in range(NPR):
            ps, bps = nextps()
            P.op("pe", lambda e, a=a, ps=ps: e.transpose(out=ps[:, 0:64], in_=stt[0:64, a * 128:(a + 1) * 128], identity=C("ident")[0:64, 0:64]), reads=[bstt, bcs], writes=[bps])
            P.op("act", lambda e, a=a, ps=ps: e.activation(out=ST[:, a, :], in_=ps[:, 0:64], func=AF.Copy), reads=[bps], writes=[bST])
            P.op("act", lambda e, a=a, ps=ps: e.activation(out=STb[:, a, :], in_=ps[:, 0:64], func=AF.Copy), reads=[bps], writes=[bSTb])

    def store_state(dst):
        for a in range(NPR):
            ps, bps = nextps()
            P.op("pe", lambda e, a=a, ps=ps: e.transpose(out=ps[0:64, 0:128], in_=ST[:, a, :], identity=C("ident")), reads=[bST, bcs], writes=[bps])
            t_, bt_ = F()
            P.op("act", lambda e, ps=ps, t_=t_: e.activation(out=t_[0:64, 0:128], in_=ps[0:64, 0:128], func=AF.Copy), reads=[bps], writes=[bt_])
            for par in range(2):
                P.dma(lambda e, a=a, par=par, t_=t_: e.dma_start(out=dst[2 * a + par], in_=t_[0:64, 64 * par:64 * par + 64]), reads=[bt_], sem=bt_)

    def prep_group(r0, n):
        nch = n // 64
        P.dma(lambda e: e.dma_start(out=lwin[:, 0:n], in_=LW[:, r0:r0 + n]), writes=[blin], sem=blin)
        P.dma(lambda e: e.dma_start(out=sg1[:, 0:n], in_=SG[0:128, r0:r0 + n]), writes=[blin], sem=blin)
        P.dma(lambda e: e.dma_start(out=sg2[0:32, 0:n], in_=SG[128:160, r0:r0 + n]), writes=[blin], sem=blin)
        for a in range(NPR):
            cs_ = slice(128 * a, 128 * a + 128)
            xk, bxk = F(); xr, bxr = F(); xv, bxv = F()
            P.dma(lambda e: e.dma_start(out=xr[:, 0:n], in_=XM[128 * a:128 * a + 128, r0:r0 + n]), writes=[bxr], sem=bxr)
            P.dma(lambda e: e.dma_start(out=xk[:, 0:n], in_=XM[WA + 128 * a:WA + 128 * a + 128, r0:r0 + n]), writes=[bxk], sem=bxk)
            P.dma(lambda e: e.dma_start(out=xv[:, 0:n], in_=XM[2 * WA + 128 * a:2 * WA + 128 * a + 128, r0:r0 + n]), writes=[bxv], sem=bxv)
            pw, bpw = nextps(); pa, bpa = nextps(); pg, bpg = nextps()
            P.op("pe", lambda e: e.matmul(pw[:, 0:n], lhsT=lwt[0:64, cs_], rhs=lwin[0:64, 0:n], start=True, stop=True), reads=[blw, blin], writes=[bpw])
            P.op("pe", lambda e: e.matmul(pa[:, 0:n], lhsT=lwt[64:128, cs_], rhs=lwin[64:128, 0:n], start=True, stop=True), reads=[blw, blin], writes=[bpa])
            P.op("pe", lambda e: e.matmul(pg[:, 0:n], lhsT=gu1[:, cs_], rhs=sg1[:, 0:n], start=True, stop=False), reads=[blw, blin], writes=[bpg])
            P.op("pe", lambda e: e.matmul(pg[:, 0:n], lhsT=gu2[0:32, cs_], rhs=sg2[0:32, 0:n], start=False, stop=True), reads=[blw, blin], writes=[bpg])
            lw_, blw_ = F(); av, bav = F()
            P.op("act", lambda e: e.activation(out=lw_[:, 0:n], in_=pw[:, 0:n], func=AF.Sigmoid, bias=prm[:, a, 0:1]), reads=[bpw, bprm], writes=[blw_])
            P.op("act", lambda e: e.activation(out=av[:, 0:n], in_=pa[:, 0:n], func=AF.Sigmoid, bias=prm[:, a, 1:2]), reads=[bpa, bprm], writes=[bav])
            P.op("act", lambda e: e.activation(out=GG[:, a, 0:n], in_=pg[:, 0:n], func=AF.Copy), reads=[bpg], writes=[bGG])
            cs, bcs2 = F()
            P.op("dve", lambda e: e.tensor_tensor_scan(out=cs[:, 0:n], data0=C("rst")[:, 0:n], data1=lw_[:, 0:n], initial=0.0, op0=ALU.mult, op1=ALU.add), reads=[bcs, blw_], writes=[bcs2])
            dd, bdd = F()
            P.op("pool", lambda e: e.tensor_tensor(out=dd[:, 0:n], in0=cs[:, 0:n], in1=lw_[:, 0:n], op=ALU.subtract), reads=[bcs2, blw_], writes=[bdd])
            kk, bkk = F()
            P.op("dve", lambda e: e.tensor_scalar(out=kk[:, 0:n], in0=xk[:, 0:n], scalar1=prm[:, a, 2:3], scalar2=None, op0=ALU.mult), reads=[bxk, bprm], writes=[bkk])
            k2, bk2 = Hh()
            P.op("act", lambda e: e.activation(out=k2[:, 0:n], in_=kk[:, 0:n], func=AF.Square), reads=[bkk], writes=[bk2])
            pss, bpss = nextps()
            P.op("pe", lambda e: e.matmul(pss[:, 0:n], lhsT=C("bones", True), rhs=k2[:, 0:n], start=True, stop=True), reads=[bcs, bk2], writes=[bpss])
            rn, brn = F()
            P.op("dve", lambda e: e.tensor_scalar(out=rn[:, 0:n], in0=pss[:, 0:n], scalar1=1e-24, scalar2=None, op0=ALU.add), reads=[bpss], writes=[brn])
            E1, bE1 = F(); E2, bE2 = F(); E3, bE3 = F()
            P.op("act", lambda e: e.activation(out=rn[:, 0:n], in_=rn[:, 0:n], func=AF.Ln), reads=[brn], writes=[brn])
            P.op("act", lambda e: e.activation(out=rn[:, 0:n], in_=rn[:, 0:n], func=AF.Exp, scale=-0.5), reads=[brn], writes=[brn])
            P.op("act", lambda e: e.activation(out=E1[:, 0:n], in_=cs[:, 0:n], func=AF.Exp, scale=-KAP), reads=[bcs2], writes=[bE1])
            P.op("act", lambda e: e.activation(out=E3[:, 0:n], in_=cs[:, 0:n], func=AF.Exp, scale=KAP), reads=[bcs2], writes=[bE3])
            P.op("act", lambda e: e.activation(out=E2[:, 0:n], in_=dd[:, 0:n], func=AF.Exp, scale=-KAP), reads=[bdd], writes=[bE2])
            P.op("act", lambda e: e.activation(out=PLc[:, a, 0:nch], in_=E1[:, 63:n:64], func=AF.Copy), reads=[bE1], writes=[bPL])
            P.op("dve", lambda e: e.tensor_tensor(out=kk[:, 0:n], in0=kk[:, 0:n], in1=rn[:, 0:n], op=ALU.mult), reads=[bkk, brn], writes=[bkk])
            kf, bkf = F()
            P.op("dve", lambda e: e.tensor_scalar(out=kf[:, 0:n], in0=av[:, 0:n], scalar1=prm[:, a, 3:4], scalar2=prm[:, a, 7:8], op0=ALU.mult, op1=ALU.add), reads=[bav, bprm], writes=[bkf])
            P.op("pool", lambda e: e.tensor_tensor(out=kf[:, 0:n], in0=kf[:, 0:n], in1=xk[:, 0:n], op=ALU.mult), reads=[bkf, bxk], writes=[bkf])
            P.op("dve", lambda e: e.scalar_tensor_tensor(out=AR[:, a, 0:nch, 0:64], in0=kk[:, 0:n].rearrange("p (c t) -> p c t", t=64), scalar=-1.0, in1=E2[:, 0:n].rearrange("p (c t) -> p c t", t=64), op0=ALU.mult, op1=ALU.mult),
                 reads=[bkk, bE2], writes=[bAR])
            P.op("pool", lambda e: e.tensor_tensor(out=AR[:, a, 0:nch, 64:128], in0=xr[:, 0:n].rearrange("p (c t) -> p c t", t=64), in1=E1[:, 0:n].rearrange("p (c t) -> p c t", t=64), op=ALU.mult),
                 reads=[bxr, bE1], writes=[bAR])
            bt32, bbt32 = F(); kt32, bkt32 = F()
            P.op("pool", lambda e: e.tensor_tensor(out=bt32[:, 0:n], in0=kk[:, 0:n], in1=av[:, 0:n], op=ALU.mult), reads=[bkk, bav], writes=[bbt32])
            P.op("dve", lambda e: e.tensor_tensor(out=bt32[:, 0:n], in0=bt32[:, 0:n], in1=E3[:, 0:n], op=ALU.mult), reads=[bbt32, bE3], writes=[bbt32])
            P.op("pool", lambda e: e.tensor_tensor(out=kt32[:, 0:n], in0=kf[:, 0:n], in1=E3[:, 0:n], op=ALU.mult), reads=[bkf, bE3], writes=[bkt32])
            P.op("act", lambda e: e.activation(out=BK[:, a, 0:nch, 0:64], in_=bt32[:, 0:n].rearrange("p (c t) -> p c t", t=64), func=AF.Copy), reads=[bbt32], writes=[bBK])
            P.op("act", lambda e: e.activation(out=BK[:, a, 0:nch, 64:128], in_=kt32[:, 0:n].rearrange("p (c t) -> p c t", t=64), func=AF.Copy), reads=[bkt32], writes=[bBK])
            rk, brk_ = Hh()
            P.op("dve", lambda e: e.tensor_tensor(out=rk[:, 0:n], in0=xr[:, 0:n], in1=kf[:, 0:n], op=ALU.mult), reads=[bxr, bkf], writes=[brk_])
            pb_, bpb_ = nextps()
            P.op("pe", lambda e: e.matmul(pb_[:, 0:n], lhsT=brk[:, a, :], rhs=rk[:, 0:n], start=True, stop=True), reads=[bbrk, brk_], writes=[bpb_])
            P.op("dve", lambda e: e.tensor_tensor(out=BON[:, a, 0:n], in0=pb_[:, 0:n], in1=xv[:, 0:n], op=ALU.mult), reads=[bpb_, bxv], writes=[bBON])
            for src, bsrc, dst, bdst in ((bt32, bbt32, BT, bBT), (kt32, bkt32, KK, bKK), (xv, bxv, VV, bVV)):
                for c4 in range(0, nch, 4):
                    ps, bps = nextps()
                    m4 = min(4, nch - c4)
                    for c in range(c4, c4 + m4):
                        P.op("pe", lambda e, c=c, ps=ps, src=src: e.transpose(out=ps[0:64, (c - c4) * 128:(c - c4 + 1) * 128], in_=src[:, 64 * c:64 * c + 64], identity=C("ident")),
                             reads=[bsrc, bcs], writes=[bps])
                    P.op("act", lambda e, ps=ps, dst=dst, c4=c4, m4=m4: e.activation(out=dst[0:64, c4:c4 + m4, cs_], in_=ps[0:64, 0:m4 * 128].rearrange("p (c x) -> p c x", c=m4), func=AF.Copy),
                         reads=[bps], writes=[bdst])

    def scan_chunk(c, col):
        if cfg.get("sq", 9) <= 0:
            return
        for hg in range(0, HA, HG):
            pA, bpA = nextps(); pK, bpK = nextps(); pN, bpN = nextps()
            for hh in range(HG):
                h = hg + hh; a, pb = h // 2, 64 * (h % 2)
                ar = AR[pb:pb + 64, a, c, :]
                P.op("pe", lambda e, hh=hh, ar=ar, a=a, pb=pb: e.matmul(pA[0:64, hh * 128:(hh + 1) * 128], lhsT=BK[pb:pb + 64, a, c, 0:64], rhs=ar, start=True, stop=True), reads=[bBK, bAR], writes=[bpA])
                P.op("pe", lambda e, hh=hh, ar=ar, a=a, pb=pb: e.matmul(pK[0:64, hh * 128:(hh + 1) * 128], lhsT=BK[pb:pb + 64, a, c, 64:128], rhs=ar, start=True, stop=True), reads=[bBK, bAR], writes=[bpK])
                P.op("pe", lambda e, hh=hh, a=a, pb=pb: e.matmul(pN[0:64, hh * 64:(hh + 1) * 64], lhsT=AR[pb:pb + 64, a, c, 0:64], rhs=BK[pb:pb + 64, a, c, 0:64], start=True, stop=True), reads=[bBK, bAR], writes=[bpN])
            if cfg.get("aq", 9) <= 0:
                continue
            ma3 = C("ma")[0:64, :].unsqueeze(1).to_broadcast([64, HG, 128])
            mab3 = C("mab")[0:64, 0:64].unsqueeze(1).to_broadcast([64, HG, 64])
            P.op("dve", lambda e: e.tensor_tensor(out=AmbA[0:64, hg:hg + HG, :], in0=pA[0:64, 0:HG * 128].rearrange("p (h x) -> p h x", h=HG), in1=ma3, op=ALU.mult), reads=[bpA, bcs], writes=[bAmbA])
            P.op("dve", lambda e: e.tensor_tensor(out=AmkA[0:64, hg:hg + HG, :], in0=pK[0:64, 0:HG * 128].rearrange("p (h x) -> p h x", h=HG), in1=ma3, op=ALU.mult), reads=[bpK, bcs], writes=[bAmkA])
            if cfg.get("aq", 9) <= 1:
                continue
            X, bX, XT, bXT = Xs[0], bXs[0], XTs[0], bXTs[0]
            P.op("act", lambda e: e.activation(out=X[0:64], in_=AmbA[0:64, hg:hg + HG, 0:64], func=AF.Copy), reads=[bAmbA], writes=[bX])
            P.op("dve", lambda e: e.tensor_tensor(out=XT[0:64], in0=pN[0:64, 0:HG * 64].rearrange("p (h x) -> p h x", h=HG), in1=mab3, op=ALU.mult), reads=[bpN, bcs], writes=[bXT])
            if cfg.get("aq", 9) <= 2:
                continue
            id3 = C("ident")[0:64, 0:64].unsqueeze(1).to_broadcast([64, HG, 64])
            P.op("dve", lambda e: e.tensor_tensor(out=Pm32[0:64], in0=AmbA[0:64, hg:hg + HG, 0:64], in1=id3, op=ALU.add), reads=[bAmbA, bcs], writes=[bPm32])
            P.op("act", lambda e: e.activation(out=Pm[0:64, hg:hg + HG, :], in_=Pm32[0:64], func=AF.Copy), reads=[bPm32], writes=[bPm])
            for lev in range(5 if cfg.get("sq", 9) > 1 else 0):
                Xn, bXn, XTn, bXTn = Xs[(lev + 1) % 2], bXs[(lev + 1) % 2], XTs[(lev + 1) % 2], bXTs[(lev + 1) % 2]
                p1, bp1 = nextps(); p2, bp2 = nextps()
                for hh in range(HG):
                    P.op("pe", lambda e, hh=hh: e.matmul(p1[0:64, hh * 64:(hh + 1) * 64], lhsT=XT[0:64, hh, :], rhs=X[0:64, hh, :], start=True, stop=True), reads=[bX, bXT], writes=[bp1])
                    P.op("pe", lambda e, hh=hh: e.matmul(p2[0:64, hh * 64:(hh + 1) * 64], lhsT=X[0:64, hh, :], rhs=XT[0:64, hh, :], start=True, stop=True), reads=[bX, bXT], writes=[bp2])
                P.op("act", lambda e: e.activation(out=Xn[0:64], in_=p1[0:64, 0:HG * 64].rearrange("p (h x) -> p h x", h=HG), func=AF.Copy), reads=[bp1], writes=[bXn])
                P.op("act", lambda e: e.activation(out=XTn[0:64], in_=p2[0:64, 0:HG * 64].rearrange("p (h x) -> p h x", h=HG), func=AF.Copy), reads=[bp2], writes=[bXTn])
                p3, bp3 = nextps()
                for hh in range(HG):
                    P.op("pe", lambda e, hh=hh: e.matmul(p3[0:64, hh * 64:(hh + 1) * 64], lhsT=XTn[0:64, hh, :], rhs=Pm[0:64, hg + hh, :], start=True, stop=True), reads=[bXTn, bPm], writes=[bp3])
                P.op("dve", lambda e: e.tensor_tensor(out=Pm32[0:64], in0=p3[0:64, 0:HG * 64].rearrange("p (h x) -> p h x", h=HG), in1=Pm32[0:64], op=ALU.add), reads=[bp3, bPm32], writes=[bPm32])
                P.op("act", lambda e: e.activation(out=Pm[0:64, hg:hg + HG, :], in_=Pm32[0:64], func=AF.Copy), reads=[bPm32], writes=[bPm])
                X, bX, XT, bXT = Xn, bXn, XTn, bXTn
        if cfg.get("sq", 9) <= 2:
            return
        for hg in range(0, HA, 8):
            nh = min(8, HA - hg)
            pW, bpW = nextps()
            for hh in range(nh):
                h = hg + hh; a, pb = h // 2, 64 * (h % 2)
                P.op("pe", lambda e, hh=hh, a=a, pb=pb: e.matmul(pW[0:64, hh * 64:(hh + 1) * 64], lhsT=AR[pb:pb + 64, a, c, 0:64], rhs=STb[pb:pb + 64, a, :], start=True, stop=False), reads=[bAR, bSTb], writes=[bpW])
                P.op("pe", lambda e, hh=hh, h=h: e.matmul(pW[0:64, hh * 64:(hh + 1) * 64], lhsT=AmkA[0:64, h, 0:64], rhs=VV[0:64, c, 64 * h:64 * h + 64], start=False, stop=True), reads=[bAmkA, bVV], writes=[bpW])
            P.op("act", lambda e: e.activation(out=Wt[0:64, hg:hg + nh, :], in_=pW[0:64, 0:nh * 64].rearrange("p (h x) -> p h x", h=nh), func=AF.Copy), reads=[bpW], writes=[bWt])
            pU, bpU = nextps()
            for hh in range(nh):
                h = hg + hh
                P.op("pe", lambda e, hh=hh, h=h: e.matmul(pU[0:64, hh * 64:(hh + 1) * 64], lhsT=Pm[0:64, h, :], rhs=Wt[0:64, h, :], start=True, stop=True), reads=[bPm, bWt], writes=[bpU])
            P.op("act", lambda e: e.activation(out=Ut[0:64, hg:hg + nh, :], in_=pU[0:64, 0:nh * 64].rearrange("p (h x) -> p h x", h=nh), func=AF.Copy), reads=[bpU], writes=[bUt])
        if cfg.get("sq", 9) <= 3:
            return
        for a in range(NPR):
            pY, bpY = nextps(); pS, bpS = nextps()
            for par in range(2):
                h, pb = 2 * a + par, 64 * par
                P.op("pe", lambda e: e.matmul(pY[pb:pb + 64, 0:64], lhsT=STb[pb:pb + 64, a, :], rhs=AR[pb:pb + 64, a, c, 64:128], start=True, stop=False), reads=[bSTb, bAR], writes=[bpY])
                P.op("pe", lambda e: e.matmul(pY[pb:pb + 64, 0:64], lhsT=Ut[0:64, h, :], rhs=AmbA[0:64, h, 64:128], start=False, stop=False), reads=[bUt, bAmbA], writes=[bpY])
                P.op("pe", lambda e: e.matmul(pY[pb:pb + 64, 0:64], lhsT=VV[0:64, c, 64 * h:64 * h + 64], rhs=AmkA[0:64, h, 64:128], start=False, stop=True), reads=[bVV, bAmkA], writes=[bpY])
                P.op("pe", lambda e: e.matmul(pS[pb:pb + 64, 0:64], lhsT=BT[0:64, c, 64 * h:64 * h + 64], rhs=Ut[0:64, h, :], start=True, stop=False), reads=[bBT, bUt], writes=[bpS])
                P.op("pe", lambda e: e.matmul(pS[pb:pb + 64, 0:64], lhsT=KK[0:64, c, 64 * h:64 * h + 64], rhs=VV[0:64, c, 64 * h:64 * h + 64], start=False, stop=True), reads=[bKK, bVV], writes=[bpS])
            P.op("act", lambda e: e.activation(out=YT[:, a, col:col + 64], in_=pY[:, 0:64], func=AF.Copy), reads=[bpY], writes=[bYT])
            P.op("pool", lambda e: e.tensor_scalar(out=ST[:, a, :], in0=ST[:, a, :], scalar1=PLc[:, a, c:c + 1], scalar2=None, op0=ALU.mult), reads=[bST, bPL], writes=[bST])
            P.op("dve", lambda e: e.scalar_tensor_tensor(out=ST[:, a, :], in0=pS[:, 0:64], scalar=PLc[:, a, c:c + 1], in1=ST[:, a, :], op0=ALU.mult, op1=ALU.add), reads=[bpS, bPL, bST], writes=[bST])
            P.op("act", lambda e: e.activation(out=STb[:, a, :], in_=ST[:, a, :], func=AF.Copy), reads=[bST], writes=[bSTb])

    def finish_group(r0, n):
        for a in range(NPR):
            yb, byb = Hh()
            P.op("act", lambda e: e.activation(out=yb[:, 0:n], in_=YT[:, a, 0:n], func=AF.Copy), reads=[bYT], writes=[byb])
            pm_, bpm_ = nextps()
            P.op("pe", lambda e: e.matmul(pm_[:, 0:n], lhsT=bmean, rhs=yb[:, 0:n], start=True, stop=True), reads=[bbm, byb], writes=[bpm_])
            yc, byc = F()
            P.op("dve", lambda e: e.tensor_tensor(out=yc[:, 0:n], in0=YT[:, a, 0:n], in1=pm_[:, 0:n], op=ALU.subtract), reads=[bYT, bpm_], writes=[byc])
            sq, bsq = Hh()
            P.op("act", lambda e: e.activation(out=sq[:, 0:n], in_=yc[:, 0:n], func=AF.Square), reads=[byc], writes=[bsq])
            pv_, bpv_ = nextps()
            P.op("pe", lambda e: e.matmul(pv_[:, 0:n], lhsT=bmean, rhs=sq[:, 0:n], start=True, stop=True), reads=[bbm, bsq], writes=[bpv_])
            rs, brs = F()
            P.op("dve", lambda e: e.tensor_scalar(out=rs[:, 0:n], in0=pv_[:, 0:n], scalar1=GN_EPS, scalar2=None, op0=ALU.add), reads=[bpv_], writes=[brs])
            P.op("act", lambda e: e.activation(out=rs[:, 0:n], in_=rs[:, 0:n], func=AF.Ln), reads=[brs], writes=[brs])
            P.op("act", lambda e: e.activation(out=rs[:, 0:n], in_=rs[:, 0:n], func=AF.Exp, scale=-0.5), reads=[brs], writes=[brs])
            P.op("dve", lambda e: e.tensor_tensor(out=yc[:, 0:n], in0=yc[:, 0:n], in1=rs[:, 0:n], op=ALU.mult), reads=[byc, brs], writes=[byc])
            P.op("dve", lambda e: e.tensor_scalar(out=yc[:, 0:n], in0=yc[:, 0:n], scalar1=prm[:, a, 5:6], scalar2=prm[:, a, 6:7], op0=ALU.mult, op1=ALU.add), reads=[byc, bprm], writes=[byc])
            P.op("pool", lambda e: e.tensor_tensor(out=yc[:, 0:n], in0=yc[:, 0:n], in1=BON[:, a, 0:n], op=ALU.add), reads=[byc, bBON], writes=[byc])
            i = fcnt["o"] % 2; fcnt["o"] += 1
            oa_, boa_ = oa16[i], boa[i]
            P.op("pool", lambda e: e.tensor_tensor(out=oa_[:, 0:n], in0=yc[:, 0:n], in1=GG[:, a, 0:n], op=ALU.mult), reads=[byc, bGG], writes=[boa_])
            P.dma(lambda e: e.dma_start(out=OA[128 * a:128 * a + 128, r0:r0 + n], in_=oa_[:, 0:n]), reads=[boa_], sem=boa_)
            if dbg2 is not None:
                P.op("pool", lambda e: e.tensor_tensor(out=yc[:, 0:n], in0=yc[:, 0:n], in1=GG[:, a, 0:n], op=ALU.mult), reads=[byc, bGG], writes=[byc])
                P.dma(lambda e: e.dma_start(out=dbg2[128 * a:128 * a + 128, r0:r0 + n], in_=yc[:, 0:n]), reads=[byc], sem=byc)

    load_state_zero()
    cq = cfg.get("cq", 9)
    cgroups = []
    r = 0
    while r < RP:
        n = min(256, RP - r); cgroups.append((r, n, False)); r += n
    cgroups.append((RP, 128, True))
    for (r0, n, is_s) in cgroups:
        if cq <= 1:
            break
        prep_group(r0, n)
        if cq <= 2:
            continue
        if not is_s:
            for c in range(n // 64):
                if r0 == 0 and c == 0:
                    P.op("pool", lambda e: e.memset(YT[:, :, 0:64], 0.0), writes=[bYT])
                    continue
                scan_chunk(c, 64 * c)
            if r0 + n == RP:
                store_state(stp)
        else:
            for j in range(2):
                load_state(j)
                scan_chunk(j, 64 * j)
                store_state(sts[j])
        if cq > 3:
            finish_group(r0, n)
    P.barrier()
    if upto == "C":
        return nc, P, dict(locals()), es
    A32.reset(); A16.reset()
    H2 = dscr("H2", [NR, D], F32); H2T = dscr("H2T", [D, NR], BF16); CWs = dscr("CWs", [NR, NE], F32)
    dgroups = []
    r = 128
    while r < RP:
        n = min(512, RP - r); dgroups.append((r, n, False)); r += n
    dgroups.append((RP, 128, True))
    KH = KD // 2
    g_mx = A32.get(D); b_mx = A32.get(D); bgm = P.buf("gm")
    P.dma(lambda e: e.dma_start(out=g_mx, in_=lnv["ln_mix_g"]), writes=[bgm], sem=bgm)
    P.dma(lambda e: e.dma_start(out=b_mx, in_=lnv["ln_mix_b"]), writes=[bgm], sem=bgm)
    wr32 = A32.get(KD * 36).rearrange("p (k m) -> p k m", k=KD); bwr = P.buf("wr")
    P.dma(lambda e: e.dma_start(out=wr32, in_=wr.rearrange("(k p) m -> p k m", p=128)), writes=[bwr], sem=bwr)
    brt = A32.get(36); P.dma(lambda e: e.dma_start(out=brt, in_=br), writes=[bwr], sem=bwr)
    h1 = A32.get(4 * D).rearrange("p (t d) -> p t d", t=4); bh1 = P.bufs(4, "h1")
    h2 = A32.get(D); bh2 = P.buf("h2")
    tmpD = A32.get(D); btmpD = P.buf("tmpD")
    statD = A32.get(40); bstatD = P.buf("statD")
    hT32 = A32.get(KD * 128).rearrange("p (k n) -> p k n", k=KD); bhT32 = P.buf("hT32")
    m1b = [A32.get(512) for _ in range(2)]; bm1 = P.bufs(2, "m1")
    m2b = [A32.get(512) for _ in range(2)]; bm2 = P.bufs(2, "m2")
    rt = A32.get(256); brt_ = P.buf("rt")
    OAt = A16.get(KH * 512).rearrange("p (k n) -> p k n", k=KH); OBt = A16.get(KH * 512).rearrange("p (k n) -> p k n", k=KH); bOAB = P.buf("oab")
    mT = A16.get(KD * 512).rearrange("p (k n) -> p k n", k=KD); bmT = P.buf("mT")
    wbs = [A16.get(KD * 128).rearrange("p (k m) -> p k m", k=KD) for _ in range(2)]; bwbs = P.bufs(2, "wb")
    gab = [A16.get(1024) for _ in range(2)]; bgab = P.bufs(2, "ga")
    wos = [A16.get(KD * 256).rearrange("p (k m) -> p k m", k=KD) for _ in range(2)]; bwos = P.bufs(2, "wo")
    hTb = [A16.get(KD * 128).rearrange("p (k n) -> p k n", k=KD) for _ in range(2)]; bhTb = P.bufs(2, "hTb")
    dc = dict(w=0, g=0, o=0, h=0)

    def router(row):
        pr, bpr = nextps()
        for k in range(KD):
            P.op("pe", lambda e, k=k: e.matmul(pr[:, 0:36], lhsT=hT32[:, k, :], rhs=wr32[:, k, :], start=(k == 0), stop=(k == KD - 1)), reads=[bhT32, bwr], writes=[bpr])
        lg = rt[:, 0:36]
        P.op("dve", lambda e: e.tensor_tensor(out=lg, in0=pr[:, 0:36], in1=brt, op=ALU.add), reads=[bpr, bwr], writes=[brt_])
        gl = rt[:, 0:4]; le3 = rt[:, 4:36].rearrange("p (g x) -> p g x", g=NG)
        gmax = rt[:, 40:41]; ngm = rt[:, 41:42]; sg_ = rt[:, 42:43]; pg_ = rt[:, 43:44]
        eg = rt[:, 44:48]; oh = rt[:, 48:52]; mx1 = rt[:, 52:56]; mx2 = rt[:, 56:60]; den = rt[:, 60:64]; sc = rt[:, 64:68]
        d3 = rt[:, 72:104].rearrange("p (g x) -> p g x", g=NG); im = rt[:, 104:136].rearrange("p (g x) -> p g x", g=NG)
        ew = rt[:, 136:168].rearrange("p (g x) -> p g x", g=NG); cw = rt[:, 168:200]
        R = dict(reads=[brt_], writes=[brt_])
        bc3 = lambda a: a.unsqueeze(2).to_broadcast([128, NG, EPG])
        P.op("dve", lambda e: e.tensor_reduce(out=gmax, in_=gl, axis=AX.X, op=ALU.max), **R)
        P.op("dve", lambda e: e.tensor_scalar(out=ngm, in0=gmax, scalar1=-1.0, scalar2=None, op0=ALU.mult), **R)
        P.op("act", lambda e: e.activation(out=eg, in_=gl, func=AF.Exp, bias=ngm), **R)
        P.op("dve", lambda e: e.tensor_reduce(out=sg_, in_=eg, axis=AX.X, op=ALU.add), **R)
        P.op("dve", lambda e: e.reciprocal(out=pg_, in_=sg_), **R)
        P.op("dve", lambda e: e.tensor_tensor(out=oh, in0=gl, in1=gmax.to_broadcast([128, NG]), op=ALU.is_equal), **R)
        P.op("dve", lambda e: e.tensor_reduce(out=mx1, in_=le3, axis=AX.X, op=ALU.max), **R)
        P.op("dve", lambda e: e.tensor_tensor(out=d3, in0=le3, in1=bc3(mx1), op=ALU.subtract), **R)
        P.op("dve", lambda e: e.tensor_scalar(out=im, in0=d3, scalar1=0.0, scalar2=-1e30, op0=ALU.is_equal, op1=ALU.mult), **R)
        P.op("dve", lambda e: e.tensor_tensor(out=im, in0=im, in1=d3, op=ALU.add), **R)
        P.op("dve", lambda e: e.tensor_reduce(out=mx2, in_=im, axis=AX.X, op=ALU.max), **R)
        P.op("dve", lambda e: e.tensor_tensor(out=im, in0=d3, in1=bc3(mx2), op=ALU.is_ge), **R)
        P.op("act", lambda e: e.activation(out=ew, in_=d3, func=AF.Exp), **R)
        P.op("dve", lambda e: e.tensor_tensor(out=ew, in0=ew, in1=im, op=ALU.mult), **R)
        P.op("dve", lambda e: e.tensor_reduce(out=den, in_=ew, axis=AX.X, op=ALU.add), **R)
        P.op("dve", lambda e: e.reciprocal(out=den, in_=den), **R)
        P.op("dve", lambda e: e.tensor_tensor(out=sc, in0=oh, in1=pg_.to_broadcast([128, NG]), op=ALU.mult), **R)
        P.op("dve", lambda e: e.tensor_tensor(out=sc, in0=sc, in1=den, op=ALU.mult), **R)
        P.op("dve", lambda e: e.tensor_tensor(out=cw.rearrange("p (g x) -> p g x", g=NG), in0=ew, in1=bc3(sc), op=ALU.mult), **R)
        P.dma(lambda e: e.dma_start(out=CWs[row:row + 128, :], in_=cw), reads=[brt_], sem=brt_)

    for (r0, n, is_s) in dgroups:
        nt = n // 128
        P.dma(lambda e: e.dma_start(out=OAt[:, :, 0:n], in_=OA[:, r0:r0 + n].rearrange("(k p) n -> p k n", p=128)), writes=[bOAB], sem=bOAB)
        P.dma(lambda e: e.dma_start(out=OBt[:, :, 0:n], in_=OB[:, r0:r0 + n].rearrange("(k p) n -> p k n", p=128)), writes=[bOAB], sem=bOAB)
        for ti in range(nt):
            P.dma(lambda e, ti=ti: e.dma_start(out=h1[:, ti, :], in_=H[r0 + 128 * ti:r0 + 128 * ti + 128, :]), writes=[bh1[ti]], sem=bh1[ti])
        for j in range(KD):
            s = dc["w"] % 2; dc["w"] += 1
            wb_, bwb_ = wbs[s], bwbs[s]
            P.dma(lambda e: e.dma_start(out=wb_, in_=w_branch[:, 128 * j:128 * j + 128].rearrange("(k p) m -> p k m", p=128)), writes=[bwb_], sem=bwb_, eng="pool")
            s2 = dc["g"] % 2; dc["g"] += 1
            ga_, bga_ = gab[s2], bgab[s2]
            P.dma(lambda e: e.dma_start(out=ga_[:, 0:n], in_=GT[128 * j:128 * j + 128, r0:r0 + n]), writes=[bga_], sem=bga_)
            P.dma(lambda e: e.dma_start(out=ga_[:, 512:512 + n], in_=GT[D + 128 * j:D + 128 * j + 128, r0:r0 + n]), writes=[bga_], sem=bga_)
            pA, bpA = nextps(); pB, bpB = nextps()
            for k in range(KH):
                P.op("pe", lambda e, k=k: e.matmul(pA[:, 0:n], lhsT=wb_[:, k, :], rhs=OAt[:, k, 0:n], start=(k == 0), stop=(k == KH - 1)), reads=[bwb_, bOAB], writes=[bpA])
            for k in range(KH):
                P.op("pe", lambda e, k=k: e.matmul(pB[:, 0:n], lhsT=wb_[:, KH + k, :], rhs=OBt[:, k, 0:n], start=(k == 0), stop=(k == KH - 1)), reads=[bwb_, bOAB], writes=[bpB])
            m1_, bm1_, m2_, bm2_ = m1b[s2], bm1[s2], m2b[s2], bm2[s2]
            P.op("dve", lambda e: e.tensor_tensor(out=m1_[:, 0:n], in0=pA[:, 0:n], in1=ga_[:, 0:n], op=ALU.mult), reads=[bpA, bga_], writes=[bm1_])
            P.op("dve", lambda e: e.tensor_tensor(out=m2_[:, 0:n], in0=pB[:, 0:n], in1=ga_[:, 512:512 + n], op=ALU.mult), reads=[bpB, bga_], writes=[bm2_])
            P.op("pool", lambda e, j=j: e.tensor_tensor(out=mT[:, j, 0:n], in0=m1_[:, 0:n], in1=m2_[:, 0:n], op=ALU.add), reads=[bm1_, bm2_], writes=[bmT])
        for nb in range(D // 256):
            s = dc["o"] % 2; dc["o"] += 1
            wo_, bwo_ = wos[s], bwos[s]
            P.dma(lambda e: e.dma_start(out=wo_, in_=w_out[:, 256 * nb:256 * nb + 256].rearrange("(k p) m -> p k m", p=128)), writes=[bwo_], sem=bwo_, eng="pool")
            for ti in range(nt):
                po, bpo = nextps()
                for k in range(KD):
                    P.op("pe", lambda e, k=k, ti=ti: e.matmul(po[:, 0:256], lhsT=mT[:, k, 128 * ti:128 * ti + 128], rhs=wo_[:, k, :], start=(k == 0), stop=(k == KD - 1)), reads=[bmT, bwo_], writes=[bpo])
                P.op("dve", lambda e, ti=ti: e.scalar_tensor_tensor(out=h1[:, ti, 256 * nb:256 * nb + 256], in0=h1[:, ti, 256 * nb:256 * nb + 256], scalar=ALPHA, in1=po[:, 0:256], op0=ALU.mult, op1=ALU.add),
                     reads=[bpo, bh1[ti]], writes=[bh1[ti]])
        for ti in range(nt):
            row = r0 + 128 * ti
            layer_norm(h1[:, ti, :], bh1[ti], g_mx, b_mx, bgm, h2, bh2, statD, bstatD, tmpD, btmpD)
            P.dma(lambda e: e.dma_start(out=H2[row:row + 128, :], in_=h2), reads=[bh2], sem=bh2)
            s = dc["h"] % 2; dc["h"] += 1
            hb_, bhb_ = hTb[s], bhTb[s]
            for kb in range(0, KD, 4):
                ps, bps = nextps()
                kk_ = min(KD, kb + 4) - kb
                for k in range(kb, kb + kk_):
                    P.op("pe", lambda e, k=k, ps=ps: e.transpose(out=ps[:, (k - kb) * 128:(k - kb + 1) * 128], in_=h2[:, k * 128:(k + 1) * 128], identity=C("ident")), reads=[bh2, bcs], writes=[bps])
                P.op("act", lambda e, ps=ps: e.activation(out=hT32[:, kb:kb + kk_, :], in_=ps[:, 0:kk_ * 128].rearrange("p (k n) -> p k n", k=kk_), func=AF.Copy), reads=[bps], writes=[bhT32])
            P.op("act", lambda e: e.activation(out=hb_, in_=hT32, func=AF.Copy), reads=[bhT32], writes=[bhb_])
            P.dma(lambda e: e.dma_start(out=H2T[:, row:row + 128].rearrange("(k p) n -> p k n", p=128), in_=hb_), reads=[bhb_], sem=bhb_)
            router(row)
    P.barrier()
    if upto == "Da":
        return nc, P, dict(locals()), es

    A32.reset(); A16.reset()
    KE = DE // 128
    g_ff = A32.get(D); b_ff = A32.get(D); bgf = P.buf("gf")
    P.dma(lambda e: e.dma_start(out=g_ff, in_=lnv["ln_ffn_g"]), writes=[bgf], sem=bgf)
    P.dma(lambda e: e.dma_start(out=b_ff, in_=lnv["ln_ffn_b"]), writes=[bgf], sem=bgf)
    acc = A32.get(4 * D).rearrange("p (t d) -> p t d", t=4); bacc = P.bufs(4, "acc")
    cwt = A32.get(4 * NE).rearrange("p (t e) -> p t e", t=4); bcw = P.buf("cw")
    yo = A32.get(D); byo = P.buf("yo")
    tmpE = A32.get(D); btmpE = P.buf("tmpE")
    statE = A32.get(40); bstatE = P.buf("statE")
    sgb = [A32.get(512) for _ in range(2)]; bsgb = P.bufs(2, "sg")
    x2T = A16.get(KD * 512).rearrange("p (k n) -> p k n", k=KD); bx2T = P.buf("x2T")
    Wg_ = A16.get(KD * DE).rearrange("p (k m) -> p k m", k=KD); bWg = P.buf("Wg")
    Wu_ = A16.get(KD * DE).rearrange("p (k m) -> p k m", k=KD); bWu = P.buf("Wu")
    Wd_ = A16.get(KE * D).rearrange("p (c d) -> p c d", c=KE); bWd = P.buf("Wd")
    hm = A16.get(KE * 512).rearrange("p (c n) -> p c n", c=KE); bhm = P.buf("hm")
    bWgc = P.bufs(KE, "Wgc"); bWuc = P.bufs(KE, "Wuc")
    for (r0, n, is_s) in dgroups:
        nt = n // 128
        dq = cfg.get("dq", 9)
        if dq <= 0:
            break
        P.dma(lambda e: e.dma_start(out=x2T[:, :, 0:n], in_=H2T[:, r0:r0 + n].rearrange("(k p) n -> p k n", p=128)), writes=[bx2T], sem=bx2T)
        if dq <= 1:
            continue
        P.dma(lambda e: e.dma_start(out=cwt[:, 0:nt, :], in_=CWs[r0:r0 + n, :].rearrange("(t p) e -> p t e", p=128)), writes=[bcw], sem=bcw)
        if dq <= 2:
            continue
        for ti in range(nt):
            P.dma(lambda e, ti=ti: e.dma_start(out=acc[:, ti, :], in_=H2[r0 + 128 * ti:r0 + 128 * ti + 128, :]), writes=[bacc[ti]], sem=bacc[ti])
            if dq <= 3:
                continue
            P.op("pool", lambda e, ti=ti: e.tensor_scalar(out=acc[:, ti, :], in0=acc[:, ti, :], scalar1=ALPHA, scalar2=None, op0=ALU.mult), reads=[bacc[ti]], writes=[bacc[ti]])
        if dq <= 4:
            continue
        for ex in range(cfg.get('ne', NE)):
            for c in range(KE):
                P.dma(lambda e, c=c: e.dma_start(out=Wg_[:, :, 128 * c:128 * c + 128], in_=mwg[ex][:, 128 * c:128 * c + 128].rearrange("(k p) m -> p k m", p=128)), writes=[bWgc[c]], sem=bWgc[c], eng="pool")
                P.dma(lambda e, c=c: e.dma_start(out=Wu_[:, :, 128 * c:128 * c + 128], in_=mwu[ex][:, 128 * c:128 * c + 128].rearrange("(k p) m -> p k m", p=128)), writes=[bWuc[c]], sem=bWuc[c], eng="pool")
            P.dma(lambda e: e.dma_start(out=Wd_, in_=mwd[ex].rearrange("(c p) d -> p c d", p=128)), writes=[bWd], sem=bWd, eng="pool")
            if cfg.get("dcut", 9) <= 1:
                continue
            for c in range(KE):
                pg, bpg = nextps(); pu, bpu = nextps()
                for k in range(KD):
                    P.op("pe", lambda e, k=k: e.matmul(pg[:, 0:n], lhsT=Wg_[:, k, 128 * c:128 * c + 128], rhs=x2T[:, k, 0:n], start=(k == 0), stop=(k == KD - 1)), reads=[bWgc[c], bx2T], writes=[bpg])
                for k in range(KD):
                    P.op("pe", lambda e, k=k: e.matmul(pu[:, 0:n], lhsT=Wu_[:, k, 128 * c:128 * c + 128], rhs=x2T[:, k, 0:n], start=(k == 0), stop=(k == KD - 1)), reads=[bWuc[c], bx2T], writes=[bpu])
                sg_, bsg_ = sgb[c % 2], bsgb[c % 2]
                P.op("act", lambda e: e.activation(out=sg_[:, 0:n], in_=pg[:, 0:n], func=AF.Sigmoid), reads=[bpg], writes=[bsg_])
                P.op("dve", lambda e: e.tensor_tensor(out=sg_[:, 0:n], in0=pg[:, 0:n], in1=sg_[:, 0:n], op=ALU.mult), reads=[bpg, bsg_], writes=[bsg_])
                P.op("dve", lambda e: e.tensor_tensor(out=hm[:, c, 0:n], in0=pu[:, 0:n], in1=sg_[:, 0:n], op=ALU.mult), reads=[bpu, bsg_], writes=[bhm])
            if cfg.get("dcut", 9) <= 2:
                continue
            for ti in range(nt):
                for nb in range(0, D, 512):
                    nw = min(512, D - nb)
                    py, bpy = nextps()
                    for c in range(KE):
                        P.op("pe", lambda e, c=c: e.matmul(py[:, 0:nw], lhsT=hm[:, c, 128 * ti:128 * ti + 128], rhs=Wd_[:, c, nb:nb + nw], start=(c == 0), stop=(c == KE - 1)), reads=[bhm, bWd], writes=[bpy])
                    P.op("dve", lambda e: e.scalar_tensor_tensor(out=acc[:, ti, nb:nb + nw], in0=py[:, 0:nw], scalar=cwt[:, ti, ex:ex + 1], in1=acc[:, ti, nb:nb + nw], op0=ALU.mult, op1=ALU.add),
                         reads=[bpy, bcw, bacc[ti]], writes=[bacc[ti]])
        for ti in range(nt):
            row = r0 + 128 * ti
            layer_norm(acc[:, ti, :], bacc[ti], g_ff, b_ff, bgf, yo, byo, statE, bstatE, tmpE, btmpE)
            if is_s:
                P.dma(lambda e: e.dma_start(out=y_s, in_=yo), reads=[byo], sem=byo)
            else:
                P.dma(lambda e: e.dma_start(out=y_p[row - 128:row, :], in_=yo), reads=[byo], sem=byo)
    P.barrier()
    return nc, P, dict(locals()), es


def make_inmaps(cfg, inp, ncores=8):
    D, SEQ, PAST = cfg["D"], cfg["SEQ"], cfg["PAST"]
    WA = D // 2; WB = D - WA; HA = WA // HD
    f = lambda a: np.ascontiguousarray(np.asarray(a, dtype=np.float32))
    shared = dict(
        meta=f(inp["meta_tokens"]), w_in=f(inp["w_in"][0]), mu=f(inp["rwkv_mu"][0]), w0=f(inp["rwkv_w0"][0]),
        w_up=f(inp["rwkv_w_up"][0]), a0=f(inp["rwkv_a0"][0]), a_up=f(inp["rwkv_a_up"][0]), g_up=f(inp["rwkv_g_up"][0]),
        k_k=f(inp["rwkv_k_k"][0]), k_a=f(inp["rwkv_k_a"][0]), r_k=f(inp["rwkv_r_k"][0]).reshape(-1),
        lnx_g=f(inp["rwkv_lnx_g"][0]), lnx_b=f(inp["rwkv_lnx_b"][0]),
        w_branch=f(inp["w_branch"][0]), w_out=f(inp["w_out"][0]),
        ln_in_g=f(np.broadcast_to(np.asarray(inp["ln_in_g"])[None], (128, D))), ln_in_b=f(np.broadcast_to(np.asarray(inp["ln_in_b"])[None], (128, D))), ln_mix_g=f(np.broadcast_to(np.asarray(inp["ln_mix_g"][0])[None], (128, D))), ln_mix_b=f(np.broadcast_to(np.asarray(inp["ln_mix_b"][0])[None], (128, D))),
        ln_ffn_g=f(np.broadcast_to(np.asarray(inp["ln_ffn_g"][0])[None], (128, D))), ln_ffn_b=f(np.broadcast_to(np.asarray(inp["ln_ffn_b"][0])[None], (128, D))),
        wr=f(np.concatenate([np.asarray(inp["router_group_w"][0]), np.asarray(inp["router_expert_w"][0])], axis=1)),
        br=f(np.broadcast_to(np.concatenate([np.asarray(inp["router_group_b"][0]), np.asarray(inp["router_expert_b"][0])], axis=0)[None], (128, NG + NE))),
        mwg=f(inp["moe_w_gate"][0]), mwu=f(inp["moe_w_up"][0]), mwd=f(inp["moe_w_down"][0]),
        cst=CONST_ARR,
    )
    maps = []
    xpa = np.asarray(inp["x_prompt"]); xsa = np.asarray(inp["x_sample"])
    cka = np.asarray(inp["cache_sb_k"][0]); cva = np.asarray(inp["cache_sb_v"][0])
    sta = np.asarray(inp["state_rwkv"][0]); sha = np.asarray(inp["state_rwkv_shift"][0])
    for c in range(ncores):
        m = dict(shared)
        m["xp"] = f(xpa[c % 4])
        m["xs"] = f(xsa[2 * c:2 * c + 2].reshape(128, D))
        m["ck"] = f(cka[2 * c:2 * c + 2].reshape(2, PAST, WB))
        m["cv"] = f(cva[2 * c:2 * c + 2].reshape(2, PAST, WB))
        m["st"] = f(sta[2 * c:2 * c + 2])
        m["shf"] = f(sha[2 * c:2 * c + 2].reshape(2, -1))
        maps.append(m)
    return maps


def assemble(cfg, res, ncores=8):
    D, SEQ, PAST = cfg["D"], cfg["SEQ"], cfg["PAST"]
    WA = D // 2; WB = D - WA; HA = WA // HD; HB = WB // HD
    WS = 3 * WA + RD + RA + RG
    TP = NMETA + SEQ
    g = lambda c, n: np.asarray(res[c][n], dtype=np.float32)
    y_p = np.stack([g(c, "y_p") for c in range(4)])
    y_s = np.concatenate([g(c, "y_s").reshape(2, DS, D) for c in range(ncores)])
    kp = np.stack([g(c, "kp").reshape(TP, HB, HD) for c in range(4)])[None]
    vp = np.stack([g(c, "vp").reshape(TP, HB, HD) for c in range(4)])[None]
    stp = np.stack([g(c, "stp") for c in range(4)])[None]
    shp = np.stack([g(c, "shp").reshape(1, WS) for c in range(4)])[None]
    ks = np.concatenate([g(c, "ks").reshape(2, DS, HB, HD) for c in range(ncores)])[None]
    vs = np.concatenate([g(c, "vs").reshape(2, DS, HB, HD) for c in range(ncores)])[None]
    sts = np.concatenate([g(c, "sts") for c in range(ncores)])[None]
    shs = np.concatenate([g(c, "shs").reshape(2, 1, WS) for c in range(ncores)])[None]
    return (y_p, y_s, kp, vp, stp, shp, ks, vs, sts, shs)


def kernel(**inputs):
    cfg = FULL
    nc, P, ctx, es = build(cfg)
    with es:
        P.emit()
    maps = make_inmaps(cfg, inputs)
    res = run_bass_kernel_spmd(nc, maps, core_ids=list(range(8)))
    return assemble(cfg, res.results)
```
